# Optimizing a Trainium2 kernel written in Bass

```python
import math
import jax, jax.numpy as jnp
from jax import lax
import numpy as np

D_MODEL = 1024
BATCH = 16
SEQ = 4096
DEPTH = 2

CHUNK = 64
EPS = 1e-6
GN_EPS = 1e-5
RET_HEADS = 8
RET_DK = 64
RET_DV = 64
RET_W = RET_HEADS * RET_DV
ROPE_BASE = 10000.0
DSA_HEADS = 8
DSA_DH = 64
DSA_W = DSA_HEADS * DSA_DH
DSA_RQ = 256
DSA_RKV = 128
IDX_HEADS = 8
IDX_DIM = 32
TOPK_MAX = 256
Q_BLOCK = 128
MEM_LEN = 256
MEM_HEADS = 4
MEM_DH = 128
MEM_W = MEM_HEADS * MEM_DH
REL_BUCKETS = 32
REL_MAX_DIST = 128
D_FF = 4 * D_MODEL
N_BRANCH = 3
IN_SIZES = (RET_HEADS * RET_DK, RET_HEADS * RET_DK, RET_W, RET_W,
            DSA_RQ, DSA_RKV, IDX_DIM, IDX_HEADS, MEM_W, N_BRANCH * D_MODEL)
N_IN = sum(IN_SIZES)

kernel_name = "chunk_causal_hybrid_retention_dsa_memory"


def rmsnorm(x, g):
    xf = x.astype(jnp.float32)
    y = xf * lax.rsqrt(jnp.mean(xf * xf, axis=-1, keepdims=True) + EPS)
    return (y * g.astype(jnp.float32)).astype(x.dtype)


def rope(x, pos):
    half = x.shape[-1] // 2
    freqs = ROPE_BASE ** (-jnp.arange(half, dtype=jnp.float32) / half)
    ang = pos[:, None] * freqs[None, :]
    cos = jnp.cos(ang)[None, :, None, :]
    sin = jnp.sin(ang)[None, :, None, :]
    xf = x.astype(jnp.float32)
    x1, x2 = xf[..., :half], xf[..., half:]
    return jnp.concatenate([x1 * cos - x2 * sin, x2 * cos + x1 * sin], axis=-1)


def retention(q, k, v):
    B, S, H, dk = q.shape
    dv = v.shape[-1]
    nc = S // CHUNK
    log_g = jnp.log1p(-jnp.exp2(-5.0 - jnp.arange(H, dtype=jnp.float32)))
    i = jnp.arange(CHUNK, dtype=jnp.float32)
    intra_decay = jnp.exp(log_g[:, None, None] * jnp.abs(i[:, None] - i[None, :]))
    key_decay = jnp.exp(log_g[:, None] * (CHUNK - 1 - i)[None, :])
    query_decay = jnp.exp(log_g[:, None] * (i + 1)[None, :])
    chunk_decay = jnp.exp(log_g * CHUNK)
    qc = q.reshape(B, nc, CHUNK, H, dk)
    kc = k.reshape(B, nc, CHUNK, H, dk) * (RET_DK ** -0.5)
    vc = v.astype(jnp.float32).reshape(B, nc, CHUNK, H, dv)
    scores = jnp.einsum('bnihd,bnjhd->bnhij', qc, kc) * intra_decay
    intra = jnp.einsum('bnhij,bnjhe->bnihe', scores, vc)
    kv = jnp.einsum('bnjhd,hj,bnjhe->nbhde', kc, key_decay, vc)

    def step(state, kv_n):
        return chunk_decay[None, :, None, None] * state + kv_n, state

    _, prev = lax.scan(step, jnp.zeros((B, H, dk, dv), jnp.float32), kv)
    cross = jnp.einsum('bnihd,nbhde,hi->bnihe', qc, prev, query_decay)
    o = (intra + cross).reshape(B, S, H, dv)
    mu = jnp.mean(o, axis=-1, keepdims=True)
    var = jnp.mean(jnp.square(o - mu), axis=-1, keepdims=True)
    return (o - mu) * lax.rsqrt(var + GN_EPS)


def t5_bucket(rel):
    nb = REL_BUCKETS // 2
    max_exact = nb // 2
    base = jnp.where(rel > 0, nb, 0)
    n = jnp.abs(rel)
    nf = jnp.maximum(n, 1).astype(jnp.float32)
    large = max_exact + (jnp.log(nf / max_exact) / math.log(REL_MAX_DIST / max_exact)
                         * (nb - max_exact)).astype(jnp.int32)
    large = jnp.minimum(large, nb - 1)
    return base + jnp.where(n < max_exact, n, large)


def dsa_attention(c_q, c_kv, k_idx, w_idx, w_uq, w_iq, w_uk, w_uv, rel_bias):
    B, S, _ = c_q.shape
    k_sel = min(TOPK_MAX, S // 4)
    nblk = S // Q_BLOCK
    q = jnp.einsum('btr,rhd->bthd', c_q, w_uq)
    q_lat = jnp.einsum('bthd,rhd->bthr', q, w_uk) * (DSA_DH ** -0.5)
    q_idx = jnp.einsum('btr,rhd->bthd', c_q, w_iq)
    w = w_idx * ((IDX_HEADS ** -0.5) * (IDX_DIM ** -0.5))
    key_pos = jnp.arange(S, dtype=jnp.int32)
    k_idx_f = k_idx.astype(jnp.float32)
    c_kv_f = c_kv.astype(jnp.float32)

    def to_blocks(a):
        return jnp.moveaxis(a.reshape((B, nblk, Q_BLOCK) + a.shape[2:]), 1, 0)

    def block(args):
        ql, qi, wb, t0 = args
        t = t0 + jnp.arange(Q_BLOCK, dtype=jnp.int32)
        end = (t // CHUNK + 1) * CHUNK - 1
        admissible = key_pos[None, :] <= end[:, None]
        s_idx = jnp.einsum('bqhd,bsd->bqhs', qi.astype(jnp.float32), k_idx_f)
        score = jnp.einsum('bqhs,bqh->bqs', jax.nn.relu(s_idx), wb.astype(jnp.float32))
        score = jnp.where(admissible[None], score, -jnp.inf)
        _, sel = lax.top_k(score, k_sel)
        valid = sel <= end[None, :, None]
        c_sel = jax.vmap(lambda c, ix: c[ix])(c_kv_f, sel)
        logits = jnp.einsum('bqhr,bqkr->bqhk', ql.astype(jnp.float32), c_sel)
        bias = rel_bias.astype(jnp.float32)[t5_bucket(sel - t[None, :, None])]
        logits = logits + jnp.moveaxis(bias, -1, -2)
        logits = jnp.where(valid[:, :, None, :], logits, -jnp.inf)
        p = jax.nn.softmax(logits, axis=-1)
        return jnp.einsum('bqhk,bqkr->bqhr', p, c_sel)

    starts = jnp.arange(nblk, dtype=jnp.int32) * Q_BLOCK
    o_lat = lax.map(block, (to_blocks(q_lat), to_blocks(q_idx), to_blocks(w), starts))
    o_lat = jnp.moveaxis(o_lat, 0, 1).reshape(B, S, DSA_HEADS, DSA_RKV)
    o = jnp.einsum('bthr,rhd->bthd', o_lat, w_uv.astype(jnp.float32))
    return o.reshape(B, S, DSA_W)


def memory_attention(q, mem_n, w_mem_kv):
    B, S, _ = q.shape
    kv = mem_n @ w_mem_kv
    k, v = jnp.split(kv, 2, axis=-1)
    M = mem_n.shape[1]
    qh = q.reshape(B, S, MEM_HEADS, MEM_DH).astype(jnp.float32)
    kh = k.reshape(B, M, MEM_HEADS, MEM_DH).astype(jnp.float32)
    vh = v.reshape(B, M, MEM_HEADS, MEM_DH).astype(jnp.float32)
    logits = jnp.einsum('bthd,bmhd->bhtm', qh, kh) * (MEM_DH ** -0.5)
    p = jax.nn.softmax(logits, axis=-1)
    return jnp.einsum('bhtm,bmhd->bthd', p, vh).reshape(B, S, MEM_W)


def setup_inputs(seed: int = 0) -> dict:
    key = jax.random.key(seed)
    ks = jax.random.split(key, 24)
    f32 = jnp.float32

    def nrm(k, shape, fan_in):
        return jax.random.normal(k, shape, f32) * (fan_in ** -0.5)

    def gain(k, shape):
        return 1.0 + 0.01 * jax.random.normal(k, shape, f32)

    L = DEPTH
    return {
        "x": jax.random.normal(ks[0], (BATCH, SEQ, D_MODEL), f32),
        "mem": jax.random.normal(ks[1], (BATCH, MEM_LEN, D_MODEL), f32),
        "norm1": gain(ks[2], (L, D_MODEL)),
        "w_in": nrm(ks[3], (L, D_MODEL, N_IN), D_MODEL),
        "q_norm": gain(ks[4], (L, DSA_RQ)),
        "kv_norm": gain(ks[5], (L, DSA_RKV)),
        "w_uq": nrm(ks[6], (L, DSA_RQ, DSA_HEADS, DSA_DH), DSA_RQ),
        "w_iq": nrm(ks[7], (L, DSA_RQ, IDX_HEADS, IDX_DIM), DSA_RQ),
        "w_uk": nrm(ks[8], (L, DSA_RKV, DSA_HEADS, DSA_DH), DSA_RKV),
        "w_uv": nrm(ks[9], (L, DSA_RKV, DSA_HEADS, DSA_DH), DSA_RKV),
        "mem_norm": gain(ks[10], (L, D_MODEL)),
        "w_mem_kv": nrm(ks[11], (L, D_MODEL, 2 * MEM_W), D_MODEL),
        "w_ret_o": nrm(ks[12], (L, RET_W, D_MODEL), RET_W),
        "w_dsa_o": nrm(ks[13], (L, DSA_W, D_MODEL), DSA_W),
        "w_mem_o": nrm(ks[14], (L, MEM_W, D_MODEL), MEM_W),
        "w_out": nrm(ks[15], (L, D_MODEL, D_MODEL), D_MODEL),
        "norm2": gain(ks[16], (L, D_MODEL)),
        "w_ff1": nrm(ks[17], (L, D_MODEL, D_FF), D_MODEL),
        "w_ff2": nrm(ks[18], (L, D_FF, D_MODEL), D_FF),
        "rel_bias": 0.5 * jax.random.normal(ks[19], (REL_BUCKETS, DSA_HEADS), f32),
        "final_norm": gain(ks[20], (D_MODEL,)),
    }


def reference(x, mem, norm1, w_in, q_norm, kv_norm, w_uq, w_iq, w_uk, w_uv, mem_norm, w_mem_kv,
              w_ret_o, w_dsa_o, w_mem_o, w_out, norm2, w_ff1, w_ff2, rel_bias, final_norm):
    B, S, D = x.shape
    pos = jnp.arange(S, dtype=jnp.float32)
    split_at = list(np.cumsum(IN_SIZES)[:-1])
    for l in range(DEPTH):
        h = rmsnorm(x, norm1[l])
        z = h @ w_in[l]
        (r_q, r_k, r_v, r_g, c_q, c_kv, i_k, i_w, m_q, gates) = jnp.split(z, split_at, axis=-1)
        rq = rope(r_q.reshape(B, S, RET_HEADS, RET_DK), pos)
        rk = rope(r_k.reshape(B, S, RET_HEADS, RET_DK), pos)
        ret = retention(rq, rk, r_v.reshape(B, S, RET_HEADS, RET_DV)).reshape(B, S, RET_W)
        ret = (jax.nn.silu(r_g.astype(jnp.float32)) * ret).astype(x.dtype)
        ret_b = ret @ w_ret_o[l]
        dsa = dsa_attention(rmsnorm(c_q, q_norm[l]), rmsnorm(c_kv, kv_norm[l]), i_k, i_w,
                            w_uq[l], w_iq[l], w_uk[l], w_uv[l], rel_bias)
        dsa_b = dsa.astype(x.dtype) @ w_dsa_o[l]
        memo = memory_attention(m_q, rmsnorm(mem, mem_norm[l]), w_mem_kv[l])
        mem_b = memo.astype(x.dtype) @ w_mem_o[l]
        g = jax.nn.sigmoid(gates.astype(jnp.float32)).reshape(B, S, N_BRANCH, D)
        merged = (g[:, :, 0] * ret_b + g[:, :, 1] * dsa_b + g[:, :, 2] * mem_b).astype(x.dtype)
        x = x + merged @ w_out[l]
        h2 = rmsnorm(x, norm2[l])
        x = x + jnp.square(jax.nn.relu(h2 @ w_ff1[l])) @ w_ff2[l]
    return rmsnorm(x, final_norm)
```

```python
import contextlib
import math
import numpy as np
import concourse.bass as bass
import concourse.mybir as mybir
from concourse.bass_utils import run_bass_kernel_spmd

F32 = mybir.dt.float32
BF16 = mybir.dt.bfloat16
AF = mybir.ActivationFunctionType
ALU = mybir.AluOpType
AX = mybir.AxisListType

D = 1024
N_IN = 6056
NCORES = 8
C_Q, C_K, C_V, C_G, C_CQ, C_CKV, C_IK, C_IW, C_MQ, C_G0, C_G1, C_G2 = (
    0, 512, 1024, 1536, 2048, 2304, 2432, 2464, 2472, 2984, 4008, 5032)
NEG = -30000.0


class Buf:
    __slots__ = ("name", "w", "r", "psum")

    def __init__(self, name, psum=False):
        self.name = name
        self.w = {}
        self.r = {}
        self.psum = psum


class TB:
    __slots__ = ("t", "b")

    def __init__(self, t, b):
        self.t = t
        self.b = b


class Prog:
    def __init__(self, nc, es):
        self.nc = nc
        self.es = es
        self.ops = []
        self.engs = ("pe", "act", "dve", "pool", "sp")
        self.nchan = 0
        self.phase = Buf("PHASE")

    def chan(self):
        self.nchan += 1
        return self.nchan - 1

    maxops = None

    def op(self, eng, fn, reads=(), writes=(), chan=None, mm=False, barrier=False, extra=()):
        i = len(self.ops)
        if Prog.maxops is not None and i >= Prog.maxops:
            return i - 1
        deps = set(extra)
        reads = list(reads)
        writes = list(writes)
        if barrier:
            writes.append(self.phase)
        else:
            reads.append(self.phase)
        key = eng if chan is None else ("c", chan)
        for b in reads:
            deps.update(b.w.values())
            if b.psum:
                deps.update(v for k, v in b.r.items() if k != key)
        for b in writes:
            deps.update(b.w.values())
            deps.update(b.r.values())
        if mm:
            deps = {d for d in deps if not (self.ops[d][4] and self.ops[d][0] == "pe")}
        self.ops.append([eng, fn, deps, chan, mm])
        for b in reads:
            b.r[key] = i
        for b in writes:
            b.w = {key: i}
            b.r = {}
        return i

    def dma(self, eng, out, in_, reads, writes, chan=None):
        rings = self.__dict__.setdefault("_rings", {})
        if eng not in rings:
            k = 8 if eng == "sp" else 3
            rings[eng] = [[self.chan() for _ in range(k)], 0, {}]
        ring = rings[eng]
        c = ring[0][ring[1] % len(ring[0])]
        ring[1] += 1
        extra = (ring[2][c],) if c in ring[2] else ()
        i = self.op(eng, lambda e: e.dma_start(out=out, in_=in_), reads, writes, chan=c, extra=extra)
        ring[2][c] = i
        return i

    def emit(self, final_chans=()):
        nc = self.nc
        ops = self.ops
        n = len(ops)
        has_dep = [False] * n
        for o in ops:
            for d in o[2]:
                has_dep[d] = True
        tick = [None] * n
        ecount = {e: 0 for e in self.engs}
        ccount = [0] * self.nchan
        for i, o in enumerate(ops):
            if o[3] is not None:
                ccount[o[3]] += 16
                tick[i] = (("c", o[3]), ccount[o[3]])
            elif has_dep[i]:
                ecount[o[0]] += 1
                tick[i] = (o[0], ecount[o[0]])
        sems = {}
        for e in self.engs:
            sems[e] = self.es.enter_context(nc.semaphore("s_" + e))
        for c in range(self.nchan):
            sems[("c", c)] = self.es.enter_context(nc.semaphore("c_%d" % c))
        per_eng = {e: [] for e in self.engs}
        for i, o in enumerate(ops):
            per_eng[o[0]].append(i)
        block = self.es.enter_context(nc.Block())
        final = [(("c", c), ccount[c]) for c in range(self.nchan) if ccount[c] > 0]

        def make(ename):
            def body(eobj):
                seen = {}
                for i in per_eng[ename]:
                    o = ops[i]
                    need = {}
                    for d in o[2]:
                        k, v = tick[d]
                        if need.get(k, 0) < v:
                            need[k] = v
                    for k, v in need.items():
                        if seen.get(k, 0) < v:
                            eobj.wait_ge(sems[k], v)
                            seen[k] = v
                    ins = o[1](eobj)
                    if tick[i] is not None:
                        k, v = tick[i]
                        ins.then_inc(sems[k], 16 if o[3] is not None else 1)
                if ename == "sp":
                    for k, v in final:
                        eobj.wait_ge(sems[k], v)
            return body

        block.tensor(make("pe"))
        block.scalar(make("act"))
        block.vector(make("dve"))
        block.gpsimd(make("pool"))
        block.sync(make("sp"))
        return {e: len(per_eng[e]) for e in per_eng}


def _t5_bucket(rel):
    nb = 16
    max_exact = 8
    base = np.where(rel > 0, nb, 0)
    n = np.abs(rel)
    nf = np.maximum(n, 1).astype(np.float32)
    large = max_exact + (np.log(nf / np.float32(max_exact)) / np.float32(math.log(128 / max_exact))
                         * np.float32(nb - max_exact)).astype(np.int32)
    large = np.minimum(large, nb - 1)
    return base + np.where(n < max_exact, n, large)


def make_consts(S, NIT):
    c = {}
    c["ident"] = np.eye(128, dtype=np.float32)
    c["antiI"] = np.ascontiguousarray(np.eye(128, dtype=np.float32)[::-1])
    pos = np.arange(S, dtype=np.float32)
    half = 32
    freqs = (np.float32(10000.0) ** (-np.arange(half, dtype=np.float32) / np.float32(half))).astype(np.float32)
    ang = pos[:, None] * freqs[None, :]
    cos = np.cos(ang).astype(np.float32)
    sin = np.sin(ang).astype(np.float32)
    c["cos2"] = np.concatenate([cos, cos], 1)
    c["sin2"] = np.concatenate([sin, sin], 1)
    H = 8
    log_g = np.log1p(-np.exp2(-5.0 - np.arange(H, dtype=np.float64)))
    i = np.arange(128)
    same = (i[:, None] // 64) == (i[None, :] // 64)
    dm = np.zeros((128, H, 128), np.float64)
    for h in range(H):
        dm[:, h, :] = np.where(same, np.exp(log_g[h] * np.abs(i[:, None] - i[None, :])), 0.0) * 0.125
    c["dmaskT"] = dm.astype(np.float32)
    tabA = np.zeros((128, 4, 128), np.float64)
    tabB = np.zeros((128, 4, 128), np.float64)
    g64 = np.zeros((128, 4, 64), np.float64)
    for p in range(4):
        for half_i in range(2):
            h = 2 * p + half_i
            rows = slice(64 * half_i, 64 * half_i + 64)
            qd = np.exp(log_g[h] * (np.arange(64) + 1))
            tabA[rows, p, 0:64] = qd[None, :]
            tabB[rows, p, 64:128] = qd[None, :]
            g64[rows, p, :] = np.exp(log_g[h] * 64)
    c["tabA"] = tabA.astype(np.float32)
    c["tabB"] = tabB.astype(np.float32)
    c["g64"] = g64.astype(np.float32)
    kd = np.zeros((128, H), np.float64)
    for h in range(H):
        kd[:, h] = np.exp(log_g[h] * (63 - (i % 64))) * 0.125
    c["kdec"] = kd.astype(np.float32)
    rel = np.arange(384) - 255
    bk = _t5_bucket(rel.astype(np.int32))
    oh = np.zeros((32, 384), np.float32)
    oh[bk, np.arange(384)] = 1.0
    c["oh"] = oh
    c["pw"] = np.tile((2.0 ** -np.arange(NIT + 2, dtype=np.float64))[None, :], (128, 1)).astype(np.float32)
    return c


class _Stop(Exception):
    pass


MARKS = []


def build_program(S, NSEQ, DEPTH, NIT=16, dbg=False, stop=None):
    NTS = S // 128
    NT = NSEQ * NTS
    KSEL = min(256, S // 4)
    nc = bass.Bass("TRN2", target_bir_lowering=False)
    es = contextlib.ExitStack()
    P = Prog(nc, es)

    def din(name, shape, dt=F32):
        return nc.dram_tensor(name, list(shape), dt, kind="ExternalInput").ap()

    def dscr(name, shape, dt=F32):
        return nc.dram_tensor(name, list(shape), dt, kind=("ExternalOutput" if dbg else "Internal")).ap()

    x_d = din("x", [NT * 128, D])
    mem_d = din("mem", [NSEQ * 256, D])
    w_in_d = din("w_in", [DEPTH, D, N_IN])
    w_ret_o_d = din("w_ret_o", [DEPTH, 512, D])
    w_dsa_o_d = din("w_dsa_o", [DEPTH, 512, D])
    w_mem_o_d = din("w_mem_o", [DEPTH, 512, D])
    w_out_d = din("w_out", [DEPTH, D, D])
    w_mkv_d = din("w_mem_kv", [DEPTH, D, D])
    w_ff1_d = din("w_ff1", [DEPTH, D, 4096])
    w_ff2_d = din("w_ff2", [DEPTH, 4096, D])
    w_uq_d = din("w_uq", [DEPTH, 256, 512])
    w_iq_d = din("w_iq", [DEPTH, 256, 256])
    w_ukT_d = din("w_ukT", [DEPTH, 1024, 128])
    w_uvp_d = din("w_uvp", [DEPTH, 128, 8 * 128])
    n1T_d = din("norm1T", [128, DEPTH * 8])
    n2T_d = din("norm2T", [128, DEPTH * 8])
    nmT_d = din("mem_normT", [128, DEPTH * 8])
    qnT_d = din("q_normT", [128, DEPTH * 2])
    kvn_d = din("kv_norm", [DEPTH, 128])
    fin_d = din("final_norm", [1, D])
    rb_d = din("rel_bias", [32, 8])
    ident_d = din("ident", [128, 128])
    antiI_d = din("antiI", [128, 128])
    cos_d = din("cos2", [S, 64])
    sin_d = din("sin2", [S, 64])
    dmask_d = din("dmaskT", [128, 1024])
    tabA_d = din("tabA", [128, 512])
    tabB_d = din("tabB", [128, 512])
    g64_d = din("g64", [128, 256])
    kdec_d = din("kdec", [128, 8])
    oh_d = din("oh", [32, 384])
    pw_d = din("pw", [128, NIT + 2])
    out_d = nc.dram_tensor("out", [NT * 128, D], F32, kind="ExternalOutput").ap()
    hT_d = dscr("hT_d", [NT * 128, D], BF16)
    part_d = dscr("part_d", [NT * 128, D])
    x1_d = dscr("x1_d", [NT * 128, D])
    x2_d = dscr("x2_d", [NT * 128, D])
    vrow_d = dscr("vrow_d", [8, 384])

    cnt = [0]

    def sbt(stack, name, shape, dt):
        cnt[0] += 1
        nm = "%s_%d" % (name, cnt[0])
        return TB(stack.enter_context(nc.sbuf_tensor(nm, list(shape), dt)), Buf(nm))

    def MM(out, lhsT, rhs, start, stop, rd, wr):
        P.op("pe", lambda e: e.matmul(out, lhsT=lhsT, rhs=rhs, start=start, stop=stop), rd, wr, mm=True)

    def ACT(out, in_, func, rd, wr, **kw):
        P.op("act", lambda e: e.activation(out=out, in_=in_, func=func, **kw), rd, wr)

    def TT(eng, out, in0, in1, op, rd, wr):
        P.op(eng, lambda e: e.tensor_tensor(out=out, in0=in0, in1=in1, op=op), rd, wr)

    def TS(eng, out, in0, s1, s2, op0, op1, rd, wr, **kw):
        if op1 is None:
            P.op(eng, lambda e: e.tensor_scalar(out=out, in0=in0, scalar1=s1, scalar2=None, op0=op0, **kw), rd, wr)
        else:
            P.op(eng, lambda e: e.tensor_scalar(out=out, in0=in0, scalar1=s1, scalar2=s2, op0=op0, op1=op1, **kw),
                 rd, wr)

    def STT(out, in0, scalar, in1, op0, op1, rd, wr):
        P.op("dve", lambda e: e.scalar_tensor_tensor(out=out, in0=in0, scalar=scalar, in1=in1, op0=op0, op1=op1),
             rd, wr)

    def CP(eng, out, in_, rd, wr):
        if eng == "act":
            P.op("act", lambda e: e.copy(out=out, in_=in_), rd, wr)
        else:
            P.op(eng, lambda e: e.tensor_copy(out=out, in_=in_), rd, wr)

    def RECIP(out, in_, rd, wr):
        P.op("dve", lambda e: e.reciprocal(out=out, in_=in_), rd, wr)

    def MEMSET(eng, ap, val, wr):
        P.op(eng, lambda e: e.memset(ap, val), [], wr)

    def RED(out, in_, op, rd, wr):
        P.op("dve", lambda e: e.tensor_reduce(out=out, in_=in_, op=op, axis=AX.X), rd, wr)

    def MAX8(out, in_, rd, wr):
        P.op("dve", lambda e: e.max(out=out, in_=in_), rd, wr)

    def barrier():
        P.op("pool", lambda e: e.memset(bar.t[:], 0.0), [], [bar.b], barrier=True)

    pb = []
    for i in range(8):
        t = es.enter_context(nc.psum_tensor("pb%d" % i, [128, 512], F32))
        pb.append(TB(t, Buf("pb%d" % i, psum=True)))

    def pbf(i):
        return pb[i].t[:].bitcast(BF16)

    G = es
    bar = sbt(G, "bar", [128, 1], F32)
    identf = sbt(G, "identf", [128, 128], F32)
    identb = sbt(G, "identb", [128, 128], BF16)
    onesb = sbt(G, "onesb", [128, 128], BF16)
    n1T = sbt(G, "n1T", [128, DEPTH * 8], F32)
    n2T = sbt(G, "n2T", [128, DEPTH * 8], F32)
    nmT = sbt(G, "nmT", [128, DEPTH * 8], F32)
    qnT = sbt(G, "qnT", [128, DEPTH * 2], F32)
    biasT = [sbt(G, "biasT%d" % i, [128, 8, 128], BF16) for i in range(2)]
    ss = sbt(G, "ss", [128, 1], F32)
    rstd = sbt(G, "rstd", [128, 1], F32)

    ch_c = P.chan()
    ch_w = P.chan()
    ch_ld = [P.chan() for _ in range(2)]
    ch_ld2 = [P.chan() for _ in range(2)]
    ch_ld3 = [P.chan() for _ in range(2)]
    ch_st = P.chan()
    ch_st2 = P.chan()
    ch_out = P.chan()

    P.dma("sp", identf.t[:], ident_d, [], [identf.b], ch_c)
    P.dma("sp", n1T.t[:], n1T_d, [], [n1T.b], ch_c)
    P.dma("sp", n2T.t[:], n2T_d, [], [n2T.b], ch_c)
    P.dma("sp", nmT.t[:], nmT_d, [], [nmT.b], ch_c)
    P.dma("sp", qnT.t[:], qnT_d, [], [qnT.b], ch_c)
    CP("dve", identb.t[:], identf.t[:], [identf.b], [identb.b])
    MEMSET("pool", onesb.t[:], 1.0, [onesb.b])

    def TR(out, in_, rd, wr):
        P.op("pe", lambda e: e.transpose(out=out, in_=in_, identity=identb.t[:]), rd + [identb.b], wr, mm=True)

    with contextlib.ExitStack() as L:
        rb = sbt(L, "rb", [32, 8], F32)
        oh = sbt(L, "oh", [32, 384], F32)
        anti = sbt(L, "anti", [128, 128], F32)
        vrow = sbt(L, "vrow", [8, 384], F32)
        vfar = sbt(L, "vfar", [8, 1], F32)
        xt = sbt(L, "xt", [128, 8, 128], F32)
        P.dma("sp", rb.t[:], rb_d, [], [rb.b], ch_c)
        P.dma("sp", oh.t[:], oh_d, [], [oh.b], ch_c)
        P.dma("sp", anti.t[:], antiI_d, [], [anti.b], ch_c)
        MM(pb[0].t[0:8, 0:384], rb.t[:], oh.t[:], True, True, [rb.b, oh.b], [pb[0].b])
        CP("dve", vfar.t[:], pb[0].t[0:8, 0:1], [pb[0].b], [vfar.b])
        TS("dve", vrow.t[:], pb[0].t[0:8, 0:384], vfar.t[:, 0:1], None, ALU.subtract, None, [pb[0].b, vfar.b], [vrow.b])
        vrowd_b = Buf("vrowd")
        P.dma("sp", vrow_d, vrow.t[:], [vrow.b], [vrowd_b], ch_st)
        for di in range(2):
            base = 128 if di == 0 else 0
            src = bass.AP(tensor=vrow_d.tensor, offset=base, ap=[[1, 128], [384, 8], [1, 128]])
            P.dma("sp", xt.t[:], src, [vrowd_b], [xt.b], ch_ld[0])
            for h in range(8):
                MM(pb[1 + h // 4].t[:, (h % 4) * 128:(h % 4 + 1) * 128], xt.t[:, h, :], anti.t[:], True, True,
                   [xt.b, anti.b], [pb[1 + h // 4].b])
            for hh in range(2):
                CP("act", biasT[di].t[:, hh * 4:(hh + 1) * 4, :],
                   pb[1 + hh].t[:].rearrange("p (h t) -> p h t", h=4), [pb[1 + hh].b], [biasT[di].b])
        barrier()

    def ckpt(name):
        MARKS.append((name, sum(1 for o in P.ops if o[0] == "pe")))
        if stop == name:
            raise _Stop()

    def _layers():
        for l in range(DEPTH):
            xin_d = x_d if l == 0 else x2_d
            last = (l == DEPTH - 1)

            with contextlib.ExitStack() as L:
                NC1 = 4608
                w1 = sbt(L, "w1", [128, 8, NC1], BF16)
                wro = sbt(L, "wro", [128, 4, D], BF16)
                wmo = sbt(L, "wmo", [128, 4, D], BF16)
                wkv = sbt(L, "wkv", [128, 8, D], BF16)
                colmap = [(C_Q, 2048, 0), (C_MQ, 512, 2048), (C_G0, 1024, 2560), (C_G2, 1024, 3584)]
                for (src0, ncol, dst0) in colmap:
                    for s0 in range(0, ncol, 512):
                        P.dma("pool", w1.t[:, :, dst0 + s0:dst0 + s0 + 512],
                              w_in_d[l, :, src0 + s0:src0 + s0 + 512].rearrange("(c p) n -> p c n", p=128),
                              [], [w1.b], ch_w)
                P.dma("pool", wro.t[:], w_ret_o_d[l].rearrange("(c p) n -> p c n", p=128), [], [wro.b], ch_w)
                P.dma("pool", wmo.t[:], w_mem_o_d[l].rearrange("(c p) n -> p c n", p=128), [], [wmo.b], ch_w)
                for s0 in range(0, D, 512):
                    P.dma("pool", wkv.t[:, :, s0:s0 + 512],
                          w_mkv_d[l, :, s0:s0 + 512].rearrange("(c p) n -> p c n", p=128), [], [wkv.b], ch_w)
                dmask = sbt(L, "dmask", [128, 1024], F32)
                tabA = sbt(L, "tabA", [128, 512], F32)
                tabB = sbt(L, "tabB", [128, 512], F32)
                g64 = sbt(L, "g64", [128, 256], F32)
                kdec = sbt(L, "kdec", [128, 8], F32)
                for tt_, dd_ in ((dmask, dmask_d), (tabA, tabA_d), (tabB, tabB_d), (g64, g64_d), (kdec, kdec_d)):
                    P.dma("sp", tt_.t[:], dd_, [], [tt_.b], ch_c)
                xs = [sbt(L, "xs", [128, D], F32) for _ in range(2)]
                cs = [sbt(L, "cs", [128, 64], F32) for _ in range(2)]
                sn = [sbt(L, "sn", [128, 64], F32) for _ in range(2)]
                junk = sbt(L, "junk", [128, D], BF16)
                hn = sbt(L, "hn", [128, D], BF16)
                hT = sbt(L, "hT", [128, 8, 128], BF16)
                tA = sbt(L, "tA", [128, 512], F32)
                tBm = sbt(L, "tBm", [128, 512], F32)
                q_r = sbt(L, "q_r", [128, 512], BF16)
                k_r = sbt(L, "k_r", [128, 512], BF16)
                kdA = sbt(L, "kdA", [128, 512], BF16)
                kdB = sbt(L, "kdB", [128, 512], BF16)
                v_s = sbt(L, "v_s", [128, 512], BF16)
                sg = sbt(L, "sg", [128, 512], F32)
                qT = sbt(L, "qT", [128, 512], BF16)
                qA = sbt(L, "qA", [128, 512], BF16)
                qB = sbt(L, "qB", [128, 512], BF16)
                kTe = sbt(L, "kTe", [128, 512], BF16)
                kTo = sbt(L, "kTo", [128, 512], BF16)
                sT = sbt(L, "sT", [128, 1024], BF16)
                S32 = [sbt(L, "S32", [128, 256], F32) for _ in range(2)]
                S16 = [sbt(L, "S16", [128, 2, 256], BF16) for _ in range(2)]
                for z_ in (kdA, kdB, kTe, kTo, S16[0], S16[1]):
                    MEMSET("pool", z_.t[:], 0.0, [z_.b])
                stmp = sbt(L, "stmp", [128, 256], F32)
                o_sb = sbt(L, "o_sb", [128, 512], F32)
                o_sq = tA
                st8 = sbt(L, "st8", [128, 32], F32)
                yb = tBm
                ret = sbt(L, "ret", [128, 512], BF16)
                retT = qB
                mqT = qT
                pT = sT
                rs = tBm
                memoT = qA
                g0 = o_sb
                g2 = sg
                t1 = tA
                t2 = tBm
                part = sbt(L, "part", [128, D], F32)
                memT = sbt(L, "memT", [128, 8, 256], BF16)
                KT = sbt(L, "KT", [128, 4, 256], BF16)
                Vm = sbt(L, "Vm", [128, 2, 512], BF16)

                def norm_to_T(xtile, gT, gcol0, dstT, dst_cols, bank):
                    ACT(junk.t[:], xtile.t[:], AF.Square, [xtile.b], [junk.b, ss.b], accum_out=ss.t[:])
                    TS("dve", rstd.t[:], ss.t[:], 1.0 / D, 1e-6, ALU.mult, ALU.add, [ss.b], [rstd.b])
                    ACT(rstd.t[:], rstd.t[:], AF.Sqrt, [rstd.b], [rstd.b])
                    RECIP(rstd.t[:], rstd.t[:], [rstd.b], [rstd.b])
                    TS("dve", hn.t[:], xtile.t[:], rstd.t[:, 0:1], None, ALU.mult, None, [xtile.b, rstd.b], [hn.b])
                    for c in range(8):
                        TR(pbf(bank)[:, c * 128:(c + 1) * 128], hn.t[:, c * 128:(c + 1) * 128], [hn.b], [pb[bank].b])
                    TT("dve", dstT.t[:, :, dst_cols], pbf(bank).rearrange("p (c t) -> p c t", c=8),
                       gT.t[:, gcol0:gcol0 + 8].unsqueeze(2).to_broadcast([128, 8, 128]), ALU.mult,
                       [pb[bank].b, gT.b], [dstT.b])

                def p1_load(n):
                    sl = n % 2
                    t = n % NTS
                    P.dma("sp", xs[sl].t[:], xin_d[n * 128:(n + 1) * 128, :], [], [xs[sl].b], ch_ld[sl])
                    P.dma("sp", cs[sl].t[:], cos_d[t * 128:(t + 1) * 128, :], [], [cs[sl].b], ch_ld2[sl])
                    P.dma("sp", sn[sl].t[:], sin_d[t * 128:(t + 1) * 128, :], [], [sn[sl].b], ch_ld3[sl])

                def rope(bank, dst, sl):
                    v3 = lambda ap: ap.rearrange("p (h d) -> p h d", h=8)
                    cb = cs[sl].t[:].unsqueeze(1).to_broadcast([128, 8, 64])
                    sb_ = sn[sl].t[:].unsqueeze(1).to_broadcast([128, 8, 64])
                    TT("dve", v3(tA.t[:]), v3(pb[bank].t[:]), cb, ALU.mult, [pb[bank].b, cs[sl].b], [tA.b])
                    TT("dve", v3(tBm.t[:]), v3(pb[bank].t[:]), sb_, ALU.mult, [pb[bank].b, sn[sl].b], [tBm.b])
                    TT("pool", v3(dst.t[:])[:, :, 0:32], v3(tA.t[:])[:, :, 0:32], v3(tBm.t[:])[:, :, 32:64], ALU.subtract,
                       [tA.b, tBm.b], [dst.b])
                    TT("pool", v3(dst.t[:])[:, :, 32:64], v3(tA.t[:])[:, :, 32:64], v3(tBm.t[:])[:, :, 0:32], ALU.add,
                       [tA.b, tBm.b], [dst.b])

                def mem_prologue(seq):
                    for mc in range(2):
                        r0 = seq * 256 + mc * 128
                        P.dma("sp", xs[0].t[:], mem_d[r0:r0 + 128, :], [], [xs[0].b], ch_ld[0])
                        norm_to_T(xs[0], nmT, l * 8, memT, slice(mc * 128, (mc + 1) * 128), 0)
                    for h in range(4):
                        bank = 1 + h // 2
                        for c in range(8):
                            MM(pb[bank].t[:, (h % 2) * 256:(h % 2 + 1) * 256], w_kvs(c, h * 128, 128), memT.t[:, c, :],
                               c == 0, c == 7, [wkv.b, memT.b], [pb[bank].b])
                    for hh in range(2):
                        CP("act", KT.t[:, hh * 2:(hh + 1) * 2, :], pb[1 + hh].t[:].rearrange("p (h m) -> p h m", h=2),
                           [pb[1 + hh].b], [KT.b])
                    for mc in range(2):
                        for c in range(8):
                            MM(pb[3 + mc].t[:], memT.t[:, c, mc * 128:(mc + 1) * 128], w_kvs(c, 512, 512),
                               c == 0, c == 7, [wkv.b, memT.b], [pb[3 + mc].b])
                        CP("dve", Vm.t[:, mc, :], pb[3 + mc].t[:], [pb[3 + mc].b], [Vm.b])

                def w_kvs(c, c0, n):
                    return wkv.t[:, c, c0:c0 + n]

                def p1_tile(n):
                    sl = n % 2
                    seq, t = divmod(n, NTS)
                    if t == 0:
                        MEMSET("pool", S32[0].t[:], 0.0, [S32[0].b])
                        MEMSET("pool", S16[0].t[:], 0.0, [S16[0].b])
                    norm_to_T(xs[sl], n1T, l * 8, hT, slice(0, 128), 0)
                    P.dma("sp", hT_d[n * 128:(n + 1) * 128, :], hT.t[:].rearrange("p c t -> p (c t)"), [hT.b], [], ch_st)
                    for blk, bank in ((0, 1), (1, 2), (2, 3), (3, 4)):
                        for c in range(8):
                            MM(pb[bank].t[:], hT.t[:, c, :], w1.t[:, c, blk * 512:(blk + 1) * 512], c == 0, c == 7,
                               [hT.b, w1.b], [pb[bank].b])
                    rope(1, q_r, sl)
                    rope(2, k_r, sl)
                    CP("act", v_s.t[:], pb[3].t[:], [pb[3].b], [v_s.b])
                    ACT(sg.t[:], pb[4].t[:], AF.Silu, [pb[4].b], [sg.b])
                    for kd_, r0 in ((kdA, 0), (kdB, 64)):
                        TT("pool", kd_.t[r0:r0 + 64, :].rearrange("p (h d) -> p h d", h=8),
                           k_r.t[r0:r0 + 64, :].rearrange("p (h d) -> p h d", h=8),
                           kdec.t[r0:r0 + 64, :].unsqueeze(2).to_broadcast([64, 8, 64]), ALU.mult, [k_r.b, kdec.b], [kd_.b])
                    for c in range(4):
                        TR(pbf(0)[:, c * 128:(c + 1) * 128], q_r.t[:, c * 128:(c + 1) * 128], [q_r.b], [pb[0].b])
                    for c in range(4):
                        TR(pbf(0)[:, 512 + c * 128:512 + (c + 1) * 128], k_r.t[:, c * 128:(c + 1) * 128], [k_r.b], [pb[0].b])
                    CP("act", qT.t[:], pbf(0)[:, 0:512], [pb[0].b], [qT.b])
                    TT("dve", qA.t[:], qT.t[:], tabA.t[:], ALU.mult, [qT.b, tabA.b], [qA.b])
                    TT("pool", qB.t[:], qT.t[:], tabB.t[:], ALU.mult, [qT.b, tabB.b], [qB.b])
                    CP("act", kTe.t[0:64, :], pbf(0)[0:64, 512:1024], [pb[0].b], [kTe.b])
                    CP("act", kTo.t[64:128, :], pbf(0)[64:128, 512:1024], [pb[0].b], [kTo.b])
                    for h in range(8):
                        p_, b_ = h // 2, 64 * (h % 2)
                        bank = 1 + h // 4
                        kT_ = kTe if h % 2 == 0 else kTo
                        MM(pb[bank].t[:, (h % 4) * 128:(h % 4 + 1) * 128], kT_.t[:, p_ * 128:(p_ + 1) * 128],
                           qT.t[:, p_ * 128:(p_ + 1) * 128], True, True, [kT_.b, qT.b], [pb[bank].b])
                    for hh in range(2):
                        TT("dve", sT.t[:, hh * 512:(hh + 1) * 512], pb[1 + hh].t[:], dmask.t[:, hh * 512:(hh + 1) * 512],
                           ALU.mult, [pb[1 + hh].b, dmask.b], [sT.b])
                    for ci in range(2):
                        r0 = 64 * ci
                        bank = 3 + ci
                        kd_ = kdA if ci == 0 else kdB
                        for p_ in range(4):
                            MM(pb[bank].t[:, p_ * 128:(p_ + 1) * 128], kd_.t[:, p_ * 128:(p_ + 1) * 128],
                               v_s.t[:, p_ * 128:(p_ + 1) * 128], True, True, [kd_.b, v_s.b], [pb[bank].b])
                        src, dst = S32[ci], S32[1 - ci]
                        TT("dve", stmp.t[:], src.t[:], g64.t[:], ALU.mult, [src.b, g64.b], [stmp.b])
                        kv4 = pb[bank].t[:].rearrange("p (a e) -> p a e", a=4)
                        d3 = dst.t[:].rearrange("p (a e) -> p a e", a=4)
                        s3 = stmp.t[:].rearrange("p (a e) -> p a e", a=4)
                        TT("dve", d3[0:64], s3[0:64], kv4[0:64, :, 0:64], ALU.add, [stmp.b, pb[bank].b], [dst.b])
                        TT("dve", d3[64:128], s3[64:128], kv4[64:128, :, 64:128], ALU.add, [stmp.b, pb[bank].b], [dst.b])
                        if ci == 0:
                            CP("pool", S16[1].t[0:64, 0, :], S32[1].t[0:64, :], [S32[1].b], [S16[1].b])
                            CP("pool", S16[1].t[64:128, 1, :], S32[1].t[64:128, :], [S32[1].b], [S16[1].b])
                    for h in range(8):
                        p_, b_ = h // 2, 64 * (h % 2)
                        o_ap = pb[5].t[:, h * 64:(h + 1) * 64]
                        MM(o_ap, sT.t[:, h * 128:(h + 1) * 128], v_s.t[:, h * 64:(h + 1) * 64], True, False,
                           [sT.b, v_s.b], [pb[5].b])
                        MM(o_ap, qA.t[:, p_ * 128:(p_ + 1) * 128], S16[0].t[:, h % 2, p_ * 64:(p_ + 1) * 64],
                           False, False, [qA.b, S16[0].b], [pb[5].b])
                        MM(o_ap, qB.t[:, p_ * 128:(p_ + 1) * 128], S16[1].t[:, h % 2, p_ * 64:(p_ + 1) * 64],
                           False, True, [qB.b, S16[1].b], [pb[5].b])
                    CP("pool", S16[0].t[0:64, 0, :], S32[0].t[0:64, :], [S32[0].b], [S16[0].b])
                    CP("pool", S16[0].t[64:128, 1, :], S32[0].t[64:128, :], [S32[0].b], [S16[0].b])
                    CP("act", o_sb.t[:], pb[5].t[:], [pb[5].b], [o_sb.b])
                    ACT(o_sq.t[:], pb[5].t[:], AF.Square, [pb[5].b], [o_sq.b])
                    o3 = o_sb.t[:].rearrange("p (h e) -> p h e", h=8)
                    RED(st8.t[:, 0:8], o3, ALU.add, [o_sb.b], [st8.b])
                    RED(st8.t[:, 8:16], o_sq.t[:].rearrange("p (h e) -> p h e", h=8), ALU.add, [o_sq.b], [st8.b])
                    TS("dve", st8.t[:, 16:24], st8.t[:, 0:8], 1.0 / 64, None, ALU.mult, None, [st8.b], [st8.b])
                    TT("dve", st8.t[:, 0:8], st8.t[:, 16:24], st8.t[:, 16:24], ALU.mult, [st8.b], [st8.b])
                    STT(st8.t[:, 24:32], st8.t[:, 8:16], 1.0 / 64, st8.t[:, 0:8], ALU.mult, ALU.subtract, [st8.b], [st8.b])
                    TS("dve", st8.t[:, 24:32], st8.t[:, 24:32], 1e-5, None, ALU.add, None, [st8.b], [st8.b])
                    ACT(st8.t[:, 24:32], st8.t[:, 24:32], AF.Sqrt, [st8.b], [st8.b])
                    RECIP(st8.t[:, 24:32], st8.t[:, 24:32], [st8.b], [st8.b])
                    y3 = yb.t[:].rearrange("p (h e) -> p h e", h=8)
                    TT("dve", y3, o3, st8.t[:, 16:24].unsqueeze(2).to_broadcast([128, 8, 64]), ALU.subtract,
                       [o_sb.b, st8.b], [yb.b])
                    TT("pool", y3, y3, st8.t[:, 24:32].unsqueeze(2).to_broadcast([128, 8, 64]), ALU.mult,
                       [yb.b, st8.b], [yb.b])
                    TT("pool", ret.t[:], yb.t[:], sg.t[:], ALU.mult, [yb.b, sg.b], [ret.b])
                    for c in range(4):
                        TR(pbf(0)[:, c * 128:(c + 1) * 128], ret.t[:, c * 128:(c + 1) * 128], [ret.b], [pb[0].b])
                    CP("act", retT.t[:], pbf(0)[:, 0:512], [pb[0].b], [retT.b])
                    for j in range(4):
                        for c in range(8):
                            MM(pb[1].t[:, j * 128:(j + 1) * 128], w1.t[:, c, 2048 + j * 128:2048 + (j + 1) * 128],
                               hT.t[:, c, :], c == 0, c == 7, [w1.b, hT.b], [pb[1].b])
                    CP("dve", mqT.t[:], pb[1].t[:], [pb[1].b], [mqT.b])
                    for mc in range(2):
                        for h in range(4):
                            MM(pb[2 + mc].t[:, h * 128:(h + 1) * 128], KT.t[:, h, mc * 128:(mc + 1) * 128],
                               mqT.t[:, h * 128:(h + 1) * 128], True, True, [KT.b, mqT.b], [pb[2 + mc].b])
                        ACT(pT.t[:, mc * 512:(mc + 1) * 512], pb[2 + mc].t[:], AF.Exp, [pb[2 + mc].b], [pT.b],
                            scale=128.0 ** -0.5)
                    for h in range(4):
                        for mc in range(2):
                            MM(pb[4].t[:, h * 128:(h + 1) * 128], Vm.t[:, mc, h * 128:(h + 1) * 128],
                               pT.t[:, mc * 512 + h * 128:mc * 512 + (h + 1) * 128], mc == 0, mc == 1,
                               [Vm.b, pT.b], [pb[4].b])
                    for mc in range(2):
                        MM(pb[5].t[:], onesb.t[:], pT.t[:, mc * 512:(mc + 1) * 512], mc == 0, mc == 1,
                           [onesb.b, pT.b], [pb[5].b])
                    RECIP(rs.t[:], pb[5].t[:], [pb[5].b], [rs.b])
                    TT("dve", memoT.t[:], pb[4].t[:], rs.t[:], ALU.mult, [pb[4].b, rs.b], [memoT.b])
                    for blk in range(2):
                        cs_ = slice(blk * 512, (blk + 1) * 512)
                        for c in range(4):
                            MM(pb[6].t[:], retT.t[:, c * 128:(c + 1) * 128], wro.t[:, c, cs_], c == 0, c == 3,
                               [retT.b, wro.b], [pb[6].b])
                        for c in range(4):
                            MM(pb[7].t[:], memoT.t[:, c * 128:(c + 1) * 128], wmo.t[:, c, cs_], c == 0, c == 3,
                               [memoT.b, wmo.b], [pb[7].b])
                        for c in range(8):
                            MM(pb[2].t[:], hT.t[:, c, :], w1.t[:, c, 2560 + blk * 512:2560 + (blk + 1) * 512], c == 0, c == 7,
                               [hT.b, w1.b], [pb[2].b])
                        for c in range(8):
                            MM(pb[3].t[:], hT.t[:, c, :], w1.t[:, c, 3584 + blk * 512:3584 + (blk + 1) * 512], c == 0, c == 7,
                               [hT.b, w1.b], [pb[3].b])
                        ACT(g0.t[:], pb[2].t[:], AF.Sigmoid, [pb[2].b], [g0.b])
                        ACT(g2.t[:], pb[3].t[:], AF.Sigmoid, [pb[3].b], [g2.b])
                        TT("dve", t1.t[:], pb[6].t[:], g0.t[:], ALU.mult, [pb[6].b, g0.b], [t1.b])
                        TT("dve", t2.t[:], pb[7].t[:], g2.t[:], ALU.mult, [pb[7].b, g2.b], [t2.b])
                        TT("pool", part.t[:, cs_], t1.t[:], t2.t[:], ALU.add, [t1.b, t2.b], [part.b])
                    P.dma("sp", part_d[n * 128:(n + 1) * 128, :], part.t[:], [part.b], [], ch_st2)

                ckpt("p1w")
                for n in range(NT):
                    if n % NTS == 0:
                        mem_prologue(n // NTS)
                        ckpt("p1m")
                        p1_load(n)
                    if (n + 1) % NTS != 0:
                        p1_load(n + 1)
                    p1_tile(n)
                    ckpt("p1t")
                barrier()
                ckpt("p1_%d" % l)

            with contextlib.ExitStack() as L:
                NC2 = 424 + 1024
                w2 = sbt(L, "w2", [128, 8, NC2], BF16)
                wuq = sbt(L, "wuq", [128, 2, 512], BF16)
                wiq = sbt(L, "wiq", [128, 2, 256], BF16)
                wuk = sbt(L, "wuk", [128, 8, 128], BF16)
                wuv = sbt(L, "wuv", [128, 8 * 128], BF16)
                wdo = sbt(L, "wdo", [128, 4, D], BF16)
                wo = sbt(L, "wo", [128, 8, D], BF16)
                P.dma("pool", w2.t[:, :, 0:424], w_in_d[l, :, C_CQ:C_CQ + 424].rearrange("(c p) n -> p c n", p=128),
                      [], [w2.b], ch_w)
                for s0 in range(0, 1024, 512):
                    P.dma("pool", w2.t[:, :, 424 + s0:424 + s0 + 512],
                          w_in_d[l, :, C_G1 + s0:C_G1 + s0 + 512].rearrange("(c p) n -> p c n", p=128), [], [w2.b], ch_w)
                P.dma("pool", wuq.t[:], w_uq_d[l].rearrange("(c p) n -> p c n", p=128), [], [wuq.b], ch_w)
                P.dma("pool", wiq.t[:], w_iq_d[l].rearrange("(c p) n -> p c n", p=128), [], [wiq.b], ch_w)
                P.dma("pool", wuk.t[:], w_ukT_d[l].rearrange("(c p) n -> p c n", p=128), [], [wuk.b], ch_w)
                P.dma("pool", wuv.t[:], w_uvp_d[l], [], [wuv.b], ch_w)
                P.dma("pool", wdo.t[:], w_dsa_o_d[l].rearrange("(c p) n -> p c n", p=128), [], [wdo.b], ch_w)
                for s0 in range(0, D, 512):
                    P.dma("pool", wo.t[:, :, s0:s0 + 512],
                          w_out_d[l, :, s0:s0 + 512].rearrange("(c p) n -> p c n", p=128), [], [wo.b], ch_w)
                gkv = sbt(L, "gkv", [128, 128], F32)
                P.dma("sp", gkv.t[:], kvn_d[l:l + 1, :].to_broadcast([128, 128]), [], [gkv.b], ch_c)
                pw = sbt(L, "pw", [128, NIT + 2], F32)
                P.dma("sp", pw.t[:], pw_d, [], [pw.b], ch_c)
                hTs = [sbt(L, "hTs", [128, 8, 128], BF16) for _ in range(2)]
                pts = [sbt(L, "pts", [128, D], F32) for _ in range(2)]
                xs = [sbt(L, "xs2", [128, D], F32) for _ in range(2)]
                ckv = sbt(L, "ckv", [128, NTS, 128], BF16)
                ckvT = sbt(L, "ckvT", [128, S], BF16)
                kiT = sbt(L, "kiT", [32, S], BF16)
                acc = sbt(L, "acc", [128, S], F32)
                nm = sbt(L, "nm", [128, S], BF16)
                nmT_ = sbt(L, "nmT_", [128, NTS, 128], BF16)
                rt = [sbt(L, "rt", [128, 512], F32) for _ in range(2)]
                cqT = sbt(L, "cqT", [128, 2, 128], BF16)
                sq = sbt(L, "sq", [128, 2, 128], BF16)
                rq8 = sbt(L, "rq8", [128, 128], F32)
                qTd = sbt(L, "qTd", [128, 4, 128], BF16)
                qlat2 = [sbt(L, "qlat", [128, 8, 128], BF16) for _ in range(2)]
                qiT = sbt(L, "qiT", [32, 8, 128], BF16)
                iw = sbt(L, "iw", [128, 8], F32)
                jk = sbt(L, "jk", [128, 128], F32)
                g1s = [sbt(L, "g1", [128, D], F32) for _ in range(2)]
                th = sbt(L, "th", [128, 8], F32)
                mx8 = sbt(L, "mx8", [128, 8], F32)
                steps = sbt(L, "steps", [128, NIT + 2], F32)
                pTs = [sbt(L, "pTs", [128, 512], BF16) for _ in range(2)]
                rs2 = sbt(L, "rs2", [128, 512], F32)
                olat = sbt(L, "olat", [128, 8, 128], BF16)
                dsaT = sbt(L, "dsaT", [128, 4, 128], BF16)
                m1 = sbt(L, "m1", [128, D], F32)
                mg = sbt(L, "mg", [128, D], BF16)
                mT = sbt(L, "mT", [128, 8, 128], BF16)
                x1 = sbt(L, "x1", [128, D], F32)

                def p2_load_h(n):
                    sl = n % 2
                    P.dma("sp", hTs[sl].t[:].rearrange("p c t -> p (c t)"), hT_d[n * 128:(n + 1) * 128, :], [],
                          [hTs[sl].b], ch_ld[sl])

                def p2_load_px(n):
                    sl = n % 2
                    P.dma("sp", pts[sl].t[:], part_d[n * 128:(n + 1) * 128, :], [], [pts[sl].b], ch_ld2[sl])
                    P.dma("sp", xs[sl].t[:], xin_d[n * 128:(n + 1) * 128, :], [], [xs[sl].b], ch_ld3[sl])

                def weave(*gens):
                    gens = list(gens)
                    while gens:
                        for g in list(gens):
                            try:
                                next(g)
                            except StopIteration:
                                gens.remove(g)

                def p2_part(n, part):
                    sl = n % 2
                    seq, t = divmod(n, NTS)
                    hT = hTs[sl]
                    NK = 128 * (t + 1)
                    qlat = qlat2[sl]
                    g1 = g1s[sl]
                    if part == "A":
                        for j in range(2):
                            for c in range(8):
                                MM(pb[7].t[:, j * 128:(j + 1) * 128], w2.t[:, c, j * 128:(j + 1) * 128], hT.t[:, c, :],
                                   c == 0, c == 7, [w2.b, hT.b], [pb[7].b])
                        for j in range(2):
                            TS("dve", cqT.t[:, j, :], pb[7].t[:, j * 128:(j + 1) * 128], qnT.t[:, l * 2 + j:l * 2 + j + 1], None,
                               ALU.mult, None, [pb[7].b, qnT.b], [cqT.b])
                        ACT(sq.t[:].rearrange("p a t -> p (a t)"), pb[7].t[:, 0:256], AF.Square, [pb[7].b], [sq.b])
                        for j in range(2):
                            MM(pb[6].t[:, 0:128], onesb.t[:], sq.t[:, j, :], j == 0, j == 1, [onesb.b, sq.b], [pb[6].b])
                        TS("dve", rq8.t[:], pb[6].t[:, 0:128], 0.25, 64e-6, ALU.mult, ALU.add, [pb[6].b], [rq8.b])
                        ACT(rq8.t[:], rq8.t[:], AF.Sqrt, [rq8.b], [rq8.b])
                        RECIP(rq8.t[:], rq8.t[:], [rq8.b], [rq8.b])
                        for j in range(4):
                            for rc in range(2):
                                MM(pb[7].t[:, j * 128:(j + 1) * 128], wuq.t[:, rc, j * 128:(j + 1) * 128], cqT.t[:, rc, :],
                                   rc == 0, rc == 1, [wuq.b, cqT.b], [pb[7].b])
                        CP("act", qTd.t[:].rearrange("p a t -> p (a t)"), pb[7].t[:], [pb[7].b], [qTd.b])
                        for h in range(8):
                            p_, b_ = h // 2, 64 * (h % 2)
                            bank = 6 + h // 4
                            MM(pb[bank].t[:, (h % 4) * 128:(h % 4 + 1) * 128], wuk.t[:, (h % 2) * 4 + p_, :],
                               qTd.t[:, p_, :], True, True, [wuk.b, qTd.b], [pb[bank].b])
                        for hh in range(2):
                            TT("dve", qlat.t[:, hh * 4:(hh + 1) * 4, :], pb[6 + hh].t[:].rearrange("p (h t) -> p h t", h=4),
                               rq8.t[:].unsqueeze(1).to_broadcast([128, 4, 128]), ALU.mult, [pb[6 + hh].b, rq8.b], [qlat.b])
                        for h in range(8):
                            bank = 6 + h // 4
                            for rc in range(2):
                                MM(pb[bank].t[0:32, (h % 4) * 128:(h % 4 + 1) * 128], wiq.t[:, rc, h * 32:(h + 1) * 32],
                                   cqT.t[:, rc, :], rc == 0, rc == 1, [wiq.b, cqT.b], [pb[bank].b])
                        for hh in range(2):
                            CP("act", qiT.t[:, hh * 4:(hh + 1) * 4, :], pb[6 + hh].t[0:32, :].rearrange("p (h t) -> p h t", h=4),
                               [pb[6 + hh].b], [qiT.b])
                        for c in range(8):
                            MM(pb[7].t[:, 0:128], hT.t[:, c, :], w2.t[:, c, 256:384], c == 0, c == 7, [hT.b, w2.b], [pb[7].b])
                        ACT(jk.t[:], pb[7].t[:, 0:128], AF.Square, [pb[7].b], [jk.b, ss.b], accum_out=ss.t[:])
                        TS("dve", rstd.t[:], ss.t[:], 1.0 / 128, 1e-6, ALU.mult, ALU.add, [ss.b], [rstd.b])
                        ACT(rstd.t[:], rstd.t[:], AF.Sqrt, [rstd.b], [rstd.b])
                        RECIP(rstd.t[:], rstd.t[:], [rstd.b], [rstd.b])
                        STT(ckv.t[:, t, :], pb[7].t[:, 0:128], rstd.t[:, 0:1], gkv.t[:], ALU.mult, ALU.mult,
                            [pb[7].b, rstd.b, gkv.b], [ckv.b])
                        TR(pbf(6)[:, 0:128], ckv.t[:, t, :], [ckv.b], [pb[6].b])
                        CP("act", ckvT.t[:, t * 128:(t + 1) * 128], pbf(6)[:, 0:128], [pb[6].b], [ckvT.b])
                        for c in range(8):
                            MM(pb[7].t[0:32, 128:256], w2.t[:, c, 384:416], hT.t[:, c, :], c == 0, c == 7, [w2.b, hT.b], [pb[7].b])
                        for c in range(8):
                            MM(pb[7].t[:, 256:264], hT.t[:, c, :], w2.t[:, c, 416:424], c == 0, c == 7, [hT.b, w2.b], [pb[7].b])
                        CP("act", kiT.t[:, t * 128:(t + 1) * 128], pb[7].t[0:32, 128:256], [pb[7].b], [kiT.b])
                        CP("dve", iw.t[:], pb[7].t[:, 256:264], [pb[7].b], [iw.b])
                        for blk in range(2):
                            for c in range(8):
                                MM(pb[6].t[:], hT.t[:, c, :], w2.t[:, c, 424 + blk * 512:424 + (blk + 1) * 512], c == 0, c == 7,
                                   [hT.b, w2.b], [pb[6].b])
                            ACT(g1.t[:, blk * 512:(blk + 1) * 512], pb[6].t[:], AF.Sigmoid, [pb[6].b], [g1.b])
                    if part == "A2":
                        kb = 0
                        ctr = 0
                        while kb < NK:
                            wk = min(512, NK - kb)
                            for h in range(8):
                                r_ = ctr % 2
                                bank = 6 + r_
                                MM(pb[bank].t[:, 0:wk], qiT.t[:, h, :], kiT.t[:, kb:kb + wk], True, True, [qiT.b, kiT.b],
                                   [pb[bank].b])
                                ACT(rt[r_].t[:, 0:wk], pb[bank].t[:, 0:wk], AF.Relu, [pb[bank].b], [rt[r_].b])
                                if h == 0:
                                    TS("dve", acc.t[:, kb:kb + wk], rt[r_].t[:, 0:wk], iw.t[:, 0:1], None, ALU.mult, None,
                                       [rt[r_].b, iw.b], [acc.b])
                                else:
                                    STT(acc.t[:, kb:kb + wk], rt[r_].t[:, 0:wk], iw.t[:, h:h + 1], acc.t[:, kb:kb + wk],
                                        ALU.mult, ALU.add, [rt[r_].b, iw.b, acc.b], [acc.b])
                                ctr += 1
                                yield
                            kb += wk
                    if part == "Bd":
                        if NK > KSEL:
                            MAX8(mx8.t[:], acc.t[:, 0:NK - 64], [acc.b], [mx8.b])
                            RED(th.t[:, 1:2], acc.t[:, 0:NK - 64], ALU.min, [acc.b], [th.b])
                            MAX8(mx8.t[64:128, :], acc.t[64:128, 0:NK], [acc.b], [mx8.b])
                            MEMSET("dve", acc.t[0:64, NK - 64:NK], -1e30, [acc.b])
                            TT("dve", th.t[:, 0:1], mx8.t[:, 0:1], th.t[:, 1:2], ALU.add, [mx8.b, th.b], [th.b])
                            TS("dve", th.t[:, 0:1], th.t[:, 0:1], 0.5, None, ALU.mult, None, [th.b], [th.b])
                            TT("dve", th.t[:, 2:3], mx8.t[:, 0:1], th.t[:, 1:2], ALU.subtract, [mx8.b, th.b], [th.b])
                            TS("dve", th.t[:, 2:3], th.t[:, 2:3], 0.5000001, 1e-30, ALU.mult, ALU.add, [th.b], [th.b])
                            TS("dve", steps.t[:], pw.t[:], th.t[:, 2:3], None, ALU.mult, None, [pw.b, th.b], [steps.b])
                            for k in range(NIT):
                                TS("dve", nm.t[:, 0:NK], acc.t[:, 0:NK], th.t[:, 0:1], 0.0, ALU.is_ge, ALU.add,
                                   [acc.b, th.b], [nm.b, th.b], accum_out=th.t[:, 3:4])
                                TS("dve", th.t[:, 4:5], th.t[:, 3:4], float(KSEL), 0.5, ALU.is_ge, ALU.subtract, [th.b], [th.b])
                                STT(th.t[:, 0:1], th.t[:, 4:5], steps.t[:, k:k + 1], th.t[:, 0:1], ALU.mult, ALU.add,
                                    [th.b, steps.b], [th.b])
                            TT("dve", th.t[:, 5:6], th.t[:, 0:1], steps.t[:, NIT:NIT + 1], ALU.subtract, [th.b, steps.b], [th.b])
                        else:
                            MEMSET("dve", acc.t[0:64, NK - 64:NK], -1e30, [acc.b])
                            MEMSET("dve", th.t[:, 5:6], -1e29, [th.b])
                        TS("dve", nm.t[:, 0:NK], acc.t[:, 0:NK], th.t[:, 5:6], NEG, ALU.is_lt, ALU.mult, [acc.b, th.b], [nm.b])
                    if part == "Bp":
                        for kt0 in range(0, t + 1, 8):
                            nblk = min(8, t + 1 - kt0)
                            for i in range(nblk):
                                kt = kt0 + i
                                TR(pbf(7)[:, i * 128:(i + 1) * 128], nm.t[:, kt * 128:(kt + 1) * 128], [nm.b], [pb[7].b])
                            CP("act", nmT_.t[:, kt0:kt0 + nblk, :].rearrange("p a t -> p (a t)"), pbf(7)[:, 0:nblk * 128],
                               [pb[7].b], [nmT_.b])
                    if part == "Ca":
                        ctr = 0
                        for kt in range(t + 1):
                            for half in range(2):
                                bank = half
                                sl2 = ctr % 2
                                MM(pb[bank].t[:], ckvT.t[:, kt * 128:(kt + 1) * 128],
                                   qlat.t[:, half * 4:(half + 1) * 4, :].rearrange("p h t -> p (h t)"), True, False,
                                   [ckvT.b, qlat.b], [pb[bank].b])
                                near = kt >= t - 1
                                MM(pb[bank].t[:].rearrange("p (h t) -> p h t", h=4), identb.t[:],
                                   nmT_.t[:, kt, :].unsqueeze(1).to_broadcast([128, 4, 128]), False, not near,
                                   [identb.b, nmT_.b], [pb[bank].b])
                                if near:
                                    MM(pb[bank].t[:].rearrange("p (h t) -> p h t", h=4), identb.t[:],
                                       biasT[t - kt].t[:, half * 4:(half + 1) * 4, :], False, True, [identb.b, biasT[t - kt].b],
                                       [pb[bank].b])
                                ACT(pTs[sl2].t[:], pb[bank].t[:], AF.Exp, [pb[bank].b], [pTs[sl2].b])
                                MM(pb[2 + half].t[:], ckv.t[:, kt, :], pTs[sl2].t[:], kt == 0, kt == t, [ckv.b, pTs[sl2].b],
                                   [pb[2 + half].b])
                                MM(pb[4 + half].t[:], onesb.t[:], pTs[sl2].t[:], kt == 0, kt == t, [onesb.b, pTs[sl2].b],
                                   [pb[4 + half].b])
                                ctr += 1
                                yield
                    if part == "Ct":
                        for half in range(2):
                            RECIP(rs2.t[:], pb[4 + half].t[:], [pb[4 + half].b], [rs2.b])
                            TT("dve", olat.t[:, half * 4:(half + 1) * 4, :].rearrange("p h t -> p (h t)"), pb[2 + half].t[:],
                               rs2.t[:], ALU.mult, [pb[2 + half].b, rs2.b], [olat.b])
                        for p_ in range(4):
                            for hi in range(2):
                                h = 2 * p_ + hi
                                MM(pb[6].t[:, p_ * 128:(p_ + 1) * 128], wuv.t[:, h * 128:(h + 1) * 128], olat.t[:, h, :],
                                   hi == 0, hi == 1, [wuv.b, olat.b], [pb[6].b])
                        CP("act", dsaT.t[:].rearrange("p a t -> p (a t)"), pb[6].t[:], [pb[6].b], [dsaT.b])
                        for blk in range(2):
                            cs_ = slice(blk * 512, (blk + 1) * 512)
                            for c in range(4):
                                MM(pb[6].t[:], dsaT.t[:, c, :], wdo.t[:, c, cs_], c == 0, c == 3, [dsaT.b, wdo.b], [pb[6].b])
                            TT("dve", m1.t[:, cs_], pb[6].t[:], g1.t[:, cs_], ALU.mult, [pb[6].b, g1.b], [m1.b])
                        TT("pool", mg.t[:], m1.t[:], pts[sl].t[:], ALU.add, [m1.b, pts[sl].b], [mg.b])
                        for c in range(8):
                            TR(pbf(7)[:, c * 128:(c + 1) * 128], mg.t[:, c * 128:(c + 1) * 128], [mg.b], [pb[7].b])
                        CP("act", mT.t[:].rearrange("p c t -> p (c t)"), pbf(7)[:, :], [pb[7].b], [mT.b])
                        for blk in range(2):
                            cs_ = slice(blk * 512, (blk + 1) * 512)
                            for c in range(8):
                                MM(pb[6].t[:], mT.t[:, c, :], wo.t[:, c, cs_], c == 0, c == 7, [mT.b, wo.b], [pb[6].b])
                            TT("dve", x1.t[:, cs_], pb[6].t[:], xs[sl].t[:, cs_], ALU.add, [pb[6].b, xs[sl].b], [x1.b])
                        P.dma("sp", x1_d[n * 128:(n + 1) * 128, :], x1.t[:], [x1.b], [], ch_st)

                ckpt("p2w")
                for seq_ in range(NSEQ):
                    n0_ = seq_ * NTS
                    p2_load_h(n0_)
                    for t_ in range(NTS):
                        n = n0_ + t_
                        if t_ + 1 < NTS:
                            p2_load_h(n + 1)
                        if t_ > 0:
                            p2_load_px(n - 1)
                        weave(p2_part(n, "A"))
                        if t_ > 0:
                            weave(p2_part(n, "A2"), p2_part(n - 1, "Ca"))
                        else:
                            weave(p2_part(n, "A2"))
                        weave(p2_part(n, "Bd"))
                        weave(p2_part(n, "Bp"))
                        if t_ > 0:
                            weave(p2_part(n - 1, "Ct"))
                        ckpt("p2t%d" % n)
                    last_ = n0_ + NTS - 1
                    p2_load_px(last_)
                    weave(p2_part(last_, "Ca"))
                    weave(p2_part(last_, "Ct"))
                barrier()
                ckpt("p2_%d" % l)

            with contextlib.ExitStack() as L:
                wf1 = sbt(L, "wf1", [128, 8, 4096], BF16)
                wf2 = sbt(L, "wf2", [128, 32, D], BF16)
                for s0 in range(0, 4096, 512):
                    P.dma("pool", wf1.t[:, :, s0:s0 + 512],
                          w_ff1_d[l, :, s0:s0 + 512].rearrange("(c p) n -> p c n", p=128), [], [wf1.b], ch_w)
                for c0 in range(0, 32, 4):
                    P.dma("pool", wf2.t[:, c0:c0 + 4, :],
                          w_ff2_d[l, c0 * 128:(c0 + 4) * 128, :].rearrange("(c p) n -> p c n", p=128), [], [wf2.b], ch_w)
                xs = [sbt(L, "xs4", [128, D], F32) for _ in range(2)]
                junk = sbt(L, "junk4", [128, D], BF16)
                hn = sbt(L, "hn4", [128, D], BF16)
                h2T = sbt(L, "h2T", [128, 8, 128], BF16)
                rl = [sbt(L, "rl", [128, 512], F32) for _ in range(2)]
                uT = sbt(L, "uT", [128, 32, 128], BF16)
                x2 = sbt(L, "x2", [128, D], F32)
                fo = sbt(L, "fo", [128, D], F32)
                if last:
                    gfin = sbt(L, "gfin", [128, D], F32)
                    P.dma("sp", gfin.t[:], fin_d.to_broadcast([128, D]), [], [gfin.b], ch_c)

                def p4_load(n):
                    sl = n % 2
                    P.dma("sp", xs[sl].t[:], x1_d[n * 128:(n + 1) * 128, :], [], [xs[sl].b], ch_ld[sl])

                def p4_tile(n):
                    sl = n % 2
                    xt_ = xs[sl]
                    ACT(junk.t[:], xt_.t[:], AF.Square, [xt_.b], [junk.b, ss.b], accum_out=ss.t[:])
                    TS("dve", rstd.t[:], ss.t[:], 1.0 / D, 1e-6, ALU.mult, ALU.add, [ss.b], [rstd.b])
                    ACT(rstd.t[:], rstd.t[:], AF.Sqrt, [rstd.b], [rstd.b])
                    RECIP(rstd.t[:], rstd.t[:], [rstd.b], [rstd.b])
                    TS("dve", hn.t[:], xt_.t[:], rstd.t[:, 0:1], None, ALU.mult, None, [xt_.b, rstd.b], [hn.b])
                    for c in range(8):
                        TR(pbf(0)[:, c * 128:(c + 1) * 128], hn.t[:, c * 128:(c + 1) * 128], [hn.b], [pb[0].b])
                    TT("dve", h2T.t[:], pbf(0).rearrange("p (c t) -> p c t", c=8),
                       n2T.t[:, l * 8:l * 8 + 8].unsqueeze(2).to_broadcast([128, 8, 128]), ALU.mult, [pb[0].b, n2T.b],
                       [h2T.b])
                    for fg in range(8):
                        bank = 1 + fg % 4
                        for j in range(4):
                            fc = fg * 4 + j
                            for c in range(8):
                                MM(pb[bank].t[:, j * 128:(j + 1) * 128], wf1.t[:, c, fc * 128:(fc + 1) * 128], h2T.t[:, c, :],
                                   c == 0, c == 7, [wf1.b, h2T.b], [pb[bank].b])
                        r_ = rl[fg % 2]
                        ACT(r_.t[:], pb[bank].t[:], AF.Relu, [pb[bank].b], [r_.b])
                        TT("dve" if fg % 2 == 0 else "pool", uT.t[:, fg * 4:(fg + 1) * 4, :].rearrange("p a t -> p (a t)"),
                           r_.t[:], r_.t[:], ALU.mult, [r_.b], [uT.b])
                    for blk in range(2):
                        cs_ = slice(blk * 512, (blk + 1) * 512)
                        bank = 5 + blk
                        for fc in range(32):
                            MM(pb[bank].t[:], uT.t[:, fc, :], wf2.t[:, fc, cs_], fc == 0, fc == 31, [uT.b, wf2.b],
                               [pb[bank].b])
                        TT("dve", x2.t[:, cs_], pb[bank].t[:], xt_.t[:, cs_], ALU.add, [pb[bank].b, xt_.b], [x2.b])
                    if not last:
                        P.dma("sp", x2_d[n * 128:(n + 1) * 128, :], x2.t[:], [x2.b], [], ch_st)
                    else:
                        if dbg:
                            P.dma("sp", x2_d[n * 128:(n + 1) * 128, :], x2.t[:], [x2.b], [], ch_st)
                        ACT(junk.t[:], x2.t[:], AF.Square, [x2.b], [junk.b, ss.b], accum_out=ss.t[:])
                        TS("dve", rstd.t[:], ss.t[:], 1.0 / D, 1e-6, ALU.mult, ALU.add, [ss.b], [rstd.b])
                        ACT(rstd.t[:], rstd.t[:], AF.Sqrt, [rstd.b], [rstd.b])
                        RECIP(rstd.t[:], rstd.t[:], [rstd.b], [rstd.b])
                        STT(fo.t[:], x2.t[:], rstd.t[:, 0:1], gfin.t[:], ALU.mult, ALU.mult, [x2.b, rstd.b, gfin.b], [fo.b])
                        P.dma("sp", out_d[n * 128:(n + 1) * 128, :], fo.t[:], [fo.b], [], ch_out)

                p4_load(0)
                for n in range(NT):
                    if n + 1 < NT:
                        p4_load(n + 1)
                    p4_tile(n)
                barrier()
                ckpt("p4_%d" % l)


    try:
        ckpt("bias")
        _layers()
    except _Stop:
        pass
    stats = P.emit(final_chans=[ch_out, ch_st, ch_st2])
    return nc, es, stats


def prep_shared(inputs, S, DEPTH, NIT):
    f = lambda a: np.ascontiguousarray(np.asarray(a, dtype=np.float32))
    sh = {}
    for k in ("w_in", "w_ret_o", "w_dsa_o", "w_mem_o", "w_out", "w_mem_kv", "w_ff1", "w_ff2", "rel_bias", "kv_norm"):
        sh[k] = f(inputs[k])
    sh["w_uq"] = f(np.asarray(inputs["w_uq"]).reshape(DEPTH, 256, 512))
    sh["w_iq"] = f(np.asarray(inputs["w_iq"]).reshape(DEPTH, 256, 256))
    wuk = np.asarray(inputs["w_uk"], dtype=np.float32)
    wukT = wuk.reshape(DEPTH, 128, 512).transpose(0, 2, 1)
    eo = np.zeros((DEPTH, 2, 4, 128, 128), np.float32)
    w5 = wukT.reshape(DEPTH, 4, 2, 64, 128)
    eo[:, 0, :, 0:64, :] = w5[:, :, 0]
    eo[:, 1, :, 64:128, :] = w5[:, :, 1]
    sh["w_ukT"] = f(eo.reshape(DEPTH, 1024, 128))
    wuv = np.asarray(inputs["w_uv"], dtype=np.float32)
    pad = np.zeros((DEPTH, 128, 8, 128), np.float32)
    for h in range(8):
        pad[:, :, h, (h % 2) * 64:(h % 2) * 64 + 64] = wuv[:, :, h, :]
    sh["w_uvp"] = f(pad.reshape(DEPTH, 128, 1024))

    def colT(a, nch):
        a = np.asarray(a, dtype=np.float32).reshape(DEPTH, nch, 128)
        return f(a.transpose(2, 0, 1).reshape(128, DEPTH * nch))
    sh["norm1T"] = colT(inputs["norm1"], 8)
    sh["norm2T"] = colT(inputs["norm2"], 8)
    sh["mem_normT"] = colT(inputs["mem_norm"], 8)
    sh["q_normT"] = colT(inputs["q_norm"], 2)
    sh["final_norm"] = f(np.asarray(inputs["final_norm"]).reshape(1, D))
    c = make_consts(S, NIT)
    sh["ident"] = c["ident"]
    sh["antiI"] = c["antiI"]
    sh["cos2"] = c["cos2"]
    sh["sin2"] = c["sin2"]
    sh["dmaskT"] = f(c["dmaskT"].reshape(128, 1024))
    sh["tabA"] = f(c["tabA"].reshape(128, 512))
    sh["tabB"] = f(c["tabB"].reshape(128, 512))
    sh["g64"] = f(c["g64"].reshape(128, 256))
    sh["kdec"] = c["kdec"]
    sh["oh"] = c["oh"]
    sh["pw"] = c["pw"]
    return sh


_CACHE = {}


def run(inputs, ncores, NSEQ, S, DEPTH, NIT=16, dbg=False, stop=None):
    key = (S, NSEQ, DEPTH, NIT, dbg, stop)
    if key not in _CACHE:
        _CACHE[key] = build_program(S, NSEQ, DEPTH, NIT, dbg, stop)
    nc, es, stats = _CACHE[key]
    sh = prep_shared(inputs, S, DEPTH, NIT)
    x = np.asarray(inputs["x"], dtype=np.float32)
    mem = np.asarray(inputs["mem"], dtype=np.float32)
    in_maps = []
    for c in range(ncores):
        m = dict(sh)
        m["x"] = np.ascontiguousarray(x[c * NSEQ:(c + 1) * NSEQ].reshape(NSEQ * S, D))
        m["mem"] = np.ascontiguousarray(mem[c * NSEQ:(c + 1) * NSEQ].reshape(NSEQ * 256, D))
        in_maps.append(m)
    res = run_bass_kernel_spmd(nc, in_maps, core_ids=list(range(ncores)))
    return res


def kernel(**inputs):
    B, S, _ = inputs["x"].shape
    NSEQ = B // NCORES
    res = run(inputs, NCORES, NSEQ, S, 2)
    outs = [r["out"].reshape(NSEQ, S, D) for r in res.results]
    return np.concatenate(outs, axis=0).astype(np.float32)
```

```python
import contextlib
import math
import numpy as np
import concourse.bass as bass
import concourse.mybir as mybir
from concourse.bass_utils import run_bass_kernel_spmd

F32 = mybir.dt.float32
BF16 = mybir.dt.bfloat16
AF = mybir.ActivationFunctionType
ALU = mybir.AluOpType
AX = mybir.AxisListType

D = 1024
N_IN = 6056
NCORES = 8
C_Q, C_K, C_V, C_G, C_CQ, C_CKV, C_IK, C_IW, C_MQ, C_G0, C_G1, C_G2 = (
    0, 512, 1024, 1536, 2048, 2304, 2432, 2464, 2472, 2984, 4008, 5032)
NEG = -30000.0


class Buf:
    __slots__ = ("name", "w", "r", "psum")

    def __init__(self, name, psum=False):
        self.name = name
        self.w = {}
        self.r = {}
        self.psum = psum


class TB:
    __slots__ = ("t", "b")

    def __init__(self, t, b):
        self.t = t
        self.b = b


class Prog:
    def __init__(self, nc, es):
        self.nc = nc
        self.es = es
        self.ops = []
        self.engs = ("pe", "act", "dve", "pool", "sp")
        self.nchan = 0
        self.phase = Buf("PHASE")

    def chan(self):
        self.nchan += 1
        return self.nchan - 1

    maxops = None

    def op(self, eng, fn, reads=(), writes=(), chan=None, mm=False, barrier=False, extra=()):
        i = len(self.ops)
        if Prog.maxops is not None and i >= Prog.maxops:
            return i - 1
        deps = set(extra)
        reads = list(reads)
        writes = list(writes)
        if barrier:
            writes.append(self.phase)
        else:
            reads.append(self.phase)
        key = eng if chan is None else ("c", chan)
        for b in reads:
            deps.update(b.w.values())
            if b.psum:
                deps.update(v for k, v in b.r.items() if k != key)
        for b in writes:
            deps.update(b.w.values())
            deps.update(b.r.values())
        if mm:
            deps = {d for d in deps if not (self.ops[d][4] and self.ops[d][0] == "pe")}
        self.ops.append([eng, fn, deps, chan, mm])
        for b in reads:
            b.r[key] = i
        for b in writes:
            b.w = {key: i}
            b.r = {}
        return i

    def dma(self, eng, out, in_, reads, writes, chan=None):
        rings = self.__dict__.setdefault("_rings", {})
        if eng not in rings:
            k = 8 if eng == "sp" else 3
            rings[eng] = [[self.chan() for _ in range(k)], 0, {}]
        ring = rings[eng]
        c = ring[0][ring[1] % len(ring[0])]
        ring[1] += 1
        extra = (ring[2][c],) if c in ring[2] else ()
        i = self.op(eng, lambda e: e.dma_start(out=out, in_=in_), reads, writes, chan=c, extra=extra)
        ring[2][c] = i
        return i

    def emit(self, final_chans=()):
        nc = self.nc
        ops = self.ops
        n = len(ops)
        has_dep = [False] * n
        for o in ops:
            for d in o[2]:
                has_dep[d] = True
        tick = [None] * n
        ecount = {e: 0 for e in self.engs}
        ccount = [0] * self.nchan
        for i, o in enumerate(ops):
            if o[3] is not None:
                ccount[o[3]] += 16
                tick[i] = (("c", o[3]), ccount[o[3]])
            elif has_dep[i]:
                ecount[o[0]] += 1
                tick[i] = (o[0], ecount[o[0]])
        sems = {}
        for e in self.engs:
            sems[e] = self.es.enter_context(nc.semaphore("s_" + e))
        for c in range(self.nchan):
            sems[("c", c)] = self.es.enter_context(nc.semaphore("c_%d" % c))
        per_eng = {e: [] for e in self.engs}
        for i, o in enumerate(ops):
            per_eng[o[0]].append(i)
        block = self.es.enter_context(nc.Block())
        final = [(("c", c), ccount[c]) for c in range(self.nchan) if ccount[c] > 0]

        def make(ename):
            def body(eobj):
                seen = {}
                for i in per_eng[ename]:
                    o = ops[i]
                    need = {}
                    for d in o[2]:
                        k, v = tick[d]
                        if need.get(k, 0) < v:
                            need[k] = v
                    for k, v in need.items():
                        if seen.get(k, 0) < v:
                            eobj.wait_ge(sems[k], v)
                            seen[k] = v
                    ins = o[1](eobj)
                    if tick[i] is not None:
                        k, v = tick[i]
                        ins.then_inc(sems[k], 16 if o[3] is not None else 1)
                if ename == "sp":
                    for k, v in final:
                        eobj.wait_ge(sems[k], v)
            return body

        block.tensor(make("pe"))
        block.scalar(make("act"))
        block.vector(make("dve"))
        block.gpsimd(make("pool"))
        block.sync(make("sp"))
        return {e: len(per_eng[e]) for e in per_eng}


def _t5_bucket(rel):
    nb = 16
    max_exact = 8
    base = np.where(rel > 0, nb, 0)
    n = np.abs(rel)
    nf = np.maximum(n, 1).astype(np.float32)
    large = max_exact + (np.log(nf / np.float32(max_exact)) / np.float32(math.log(128 / max_exact))
                         * np.float32(nb - max_exact)).astype(np.int32)
    large = np.minimum(large, nb - 1)
    return base + np.where(n < max_exact, n, large)


def make_consts(S, NIT):
    c = {}
    c["ident"] = np.eye(128, dtype=np.float32)
    c["antiI"] = np.ascontiguousarray(np.eye(128, dtype=np.float32)[::-1])
    pos = np.arange(S, dtype=np.float32)
    half = 32
    freqs = (np.float32(10000.0) ** (-np.arange(half, dtype=np.float32) / np.float32(half))).astype(np.float32)
    ang = pos[:, None] * freqs[None, :]
    cos = np.cos(ang).astype(np.float32)
    sin = np.sin(ang).astype(np.float32)
    c["cos2"] = np.concatenate([cos, cos], 1)
    c["sin2"] = np.concatenate([sin, sin], 1)
    H = 8
    log_g = np.log1p(-np.exp2(-5.0 - np.arange(H, dtype=np.float64)))
    i = np.arange(128)
    same = (i[:, None] // 64) == (i[None, :] // 64)
    dm = np.zeros((128, H, 128), np.float64)
    for h in range(H):
        dm[:, h, :] = np.where(same, np.exp(log_g[h] * np.abs(i[:, None] - i[None, :])), 0.0) * 0.125
    c["dmaskT"] = dm.astype(np.float32)
    tabA = np.zeros((128, 4, 128), np.float64)
    tabB = np.zeros((128, 4, 128), np.float64)
    g64 = np.zeros((128, 4, 64), np.float64)
    for p in range(4):
        for half_i in range(2):
            h = 2 * p + half_i
            rows = slice(64 * half_i, 64 * half_i + 64)
            qd = np.exp(log_g[h] * (np.arange(64) + 1))
            tabA[rows, p, 0:64] = qd[None, :]
            tabB[rows, p, 64:128] = qd[None, :]
            g64[rows, p, :] = np.exp(log_g[h] * 64)
    c["tabA"] = tabA.astype(np.float32)
    c["tabB"] = tabB.astype(np.float32)
    c["g64"] = g64.astype(np.float32)
    kd = np.zeros((128, H), np.float64)
    for h in range(H):
        kd[:, h] = np.exp(log_g[h] * (63 - (i % 64))) * 0.125
    c["kdec"] = kd.astype(np.float32)
    rel = np.arange(384) - 255
    bk = _t5_bucket(rel.astype(np.int32))
    oh = np.zeros((32, 384), np.float32)
    oh[bk, np.arange(384)] = 1.0
    c["oh"] = oh
    c["pw"] = np.tile((2.0 ** -np.arange(NIT + 2, dtype=np.float64))[None, :], (128, 1)).astype(np.float32)
    return c


class _Stop(Exception):
    pass


MARKS = []


def build_program(S, NSEQ, DEPTH, NIT=16, dbg=False, stop=None):
    NTS = S // 128
    NT = NSEQ * NTS
    KSEL = min(256, S // 4)
    nc = bass.Bass("TRN2", target_bir_lowering=False)
    es = contextlib.ExitStack()
    P = Prog(nc, es)

    def din(name, shape, dt=F32):
        return nc.dram_tensor(name, list(shape), dt, kind="ExternalInput").ap()

    def dscr(name, shape, dt=F32):
        return nc.dram_tensor(name, list(shape), dt, kind=("ExternalOutput" if dbg else "Internal")).ap()

    x_d = din("x", [NT * 128, D])
    mem_d = din("mem", [NSEQ * 256, D])
    w_in_d = din("w_in", [DEPTH, D, N_IN])
    w_ret_o_d = din("w_ret_o", [DEPTH, 512, D])
    w_dsa_o_d = din("w_dsa_o", [DEPTH, 512, D])
    w_mem_o_d = din("w_mem_o", [DEPTH, 512, D])
    w_out_d = din("w_out", [DEPTH, D, D])
    w_mkv_d = din("w_mem_kv", [DEPTH, D, D])
    w_ff1_d = din("w_ff1", [DEPTH, D, 4096])
    w_ff2_d = din("w_ff2", [DEPTH, 4096, D])
    w_uq_d = din("w_uq", [DEPTH, 256, 512])
    w_iq_d = din("w_iq", [DEPTH, 256, 256])
    w_ukT_d = din("w_ukT", [DEPTH, 1024, 128])
    w_uvp_d = din("w_uvp", [DEPTH, 128, 8 * 128])
    n1T_d = din("norm1T", [128, DEPTH * 8])
    n2T_d = din("norm2T", [128, DEPTH * 8])
    nmT_d = din("mem_normT", [128, DEPTH * 8])
    qnT_d = din("q_normT", [128, DEPTH * 2])
    kvn_d = din("kv_norm", [DEPTH, 128])
    fin_d = din("final_norm", [1, D])
    rb_d = din("rel_bias", [32, 8])
    ident_d = din("ident", [128, 128])
    antiI_d = din("antiI", [128, 128])
    cos_d = din("cos2", [S, 64])
    sin_d = din("sin2", [S, 64])
    dmask_d = din("dmaskT", [128, 1024])
    tabA_d = din("tabA", [128, 512])
    tabB_d = din("tabB", [128, 512])
    g64_d = din("g64", [128, 256])
    kdec_d = din("kdec", [128, 8])
    oh_d = din("oh", [32, 384])
    pw_d = din("pw", [128, NIT + 2])
    out_d = nc.dram_tensor("out", [NT * 128, D], F32, kind="ExternalOutput").ap()
    hT_d = dscr("hT_d", [NT * 128, D], BF16)
    part_d = dscr("part_d", [NT * 128, D])
    x1_d = dscr("x1_d", [NT * 128, D])
    x2_d = dscr("x2_d", [NT * 128, D])
    vrow_d = dscr("vrow_d", [8, 384])

    cnt = [0]

    def sbt(stack, name, shape, dt):
        cnt[0] += 1
        nm = "%s_%d" % (name, cnt[0])
        return TB(stack.enter_context(nc.sbuf_tensor(nm, list(shape), dt)), Buf(nm))

    def MM(out, lhsT, rhs, start, stop, rd, wr):
        P.op("pe", lambda e: e.matmul(out, lhsT=lhsT, rhs=rhs, start=start, stop=stop), rd, wr, mm=True)

    def ACT(out, in_, func, rd, wr, **kw):
        P.op("act", lambda e: e.activation(out=out, in_=in_, func=func, **kw), rd, wr)

    def TT(eng, out, in0, in1, op, rd, wr):
        P.op(eng, lambda e: e.tensor_tensor(out=out, in0=in0, in1=in1, op=op), rd, wr)

    def TS(eng, out, in0, s1, s2, op0, op1, rd, wr, **kw):
        if op1 is None:
            P.op(eng, lambda e: e.tensor_scalar(out=out, in0=in0, scalar1=s1, scalar2=None, op0=op0, **kw), rd, wr)
        else:
            P.op(eng, lambda e: e.tensor_scalar(out=out, in0=in0, scalar1=s1, scalar2=s2, op0=op0, op1=op1, **kw),
                 rd, wr)

    def STT(out, in0, scalar, in1, op0, op1, rd, wr):
        P.op("dve", lambda e: e.scalar_tensor_tensor(out=out, in0=in0, scalar=scalar, in1=in1, op0=op0, op1=op1),
             rd, wr)

    def CP(eng, out, in_, rd, wr):
        if eng == "act":
            P.op("act", lambda e: e.copy(out=out, in_=in_), rd, wr)
        else:
            P.op(eng, lambda e: e.tensor_copy(out=out, in_=in_), rd, wr)

    def RECIP(out, in_, rd, wr):
        P.op("dve", lambda e: e.reciprocal(out=out, in_=in_), rd, wr)

    def MEMSET(eng, ap, val, wr):
        P.op(eng, lambda e: e.memset(ap, val), [], wr)

    def RED(out, in_, op, rd, wr):
        P.op("dve", lambda e: e.tensor_reduce(out=out, in_=in_, op=op, axis=AX.X), rd, wr)

    def MAX8(out, in_, rd, wr):
        P.op("dve", lambda e: e.max(out=out, in_=in_), rd, wr)

    def barrier():
        P.op("pool", lambda e: e.memset(bar.t[:], 0.0), [], [bar.b], barrier=True)

    pb = []
    for i in range(8):
        t = es.enter_context(nc.psum_tensor("pb%d" % i, [128, 512], F32))
        pb.append(TB(t, Buf("pb%d" % i, psum=True)))

    def pbf(i):
        return pb[i].t[:].bitcast(BF16)

    G = es
    bar = sbt(G, "bar", [128, 1], F32)
    identf = sbt(G, "identf", [128, 128], F32)
    identb = sbt(G, "identb", [128, 128], BF16)
    onesb = sbt(G, "onesb", [128, 128], BF16)
    n1T = sbt(G, "n1T", [128, DEPTH * 8], F32)
    n2T = sbt(G, "n2T", [128, DEPTH * 8], F32)
    nmT = sbt(G, "nmT", [128, DEPTH * 8], F32)
    qnT = sbt(G, "qnT", [128, DEPTH * 2], F32)
    biasT = [sbt(G, "biasT%d" % i, [128, 8, 128], BF16) for i in range(2)]
    ss = sbt(G, "ss", [128, 1], F32)
    rstd = sbt(G, "rstd", [128, 1], F32)

    ch_c = P.chan()
    ch_w = P.chan()
    ch_ld = [P.chan() for _ in range(2)]
    ch_ld2 = [P.chan() for _ in range(2)]
    ch_ld3 = [P.chan() for _ in range(2)]
    ch_st = P.chan()
    ch_st2 = P.chan()
    ch_out = P.chan()

    P.dma("sp", identf.t[:], ident_d, [], [identf.b], ch_c)
    P.dma("sp", n1T.t[:], n1T_d, [], [n1T.b], ch_c)
    P.dma("sp", n2T.t[:], n2T_d, [], [n2T.b], ch_c)
    P.dma("sp", nmT.t[:], nmT_d, [], [nmT.b], ch_c)
    P.dma("sp", qnT.t[:], qnT_d, [], [qnT.b], ch_c)
    CP("dve", identb.t[:], identf.t[:], [identf.b], [identb.b])
    MEMSET("pool", onesb.t[:], 1.0, [onesb.b])

    def TR(out, in_, rd, wr):
        P.op("pe", lambda e: e.transpose(out=out, in_=in_, identity=identb.t[:]), rd + [identb.b], wr, mm=True)

    with contextlib.ExitStack() as L:
        rb = sbt(L, "rb", [32, 8], F32)
        oh = sbt(L, "oh", [32, 384], F32)
        anti = sbt(L, "anti", [128, 128], F32)
        vrow = sbt(L, "vrow", [8, 384], F32)
        vfar = sbt(L, "vfar", [8, 1], F32)
        xt = sbt(L, "xt", [128, 8, 128], F32)
        P.dma("sp", rb.t[:], rb_d, [], [rb.b], ch_c)
        P.dma("sp", oh.t[:], oh_d, [], [oh.b], ch_c)
        P.dma("sp", anti.t[:], antiI_d, [], [anti.b], ch_c)
        MM(pb[0].t[0:8, 0:384], rb.t[:], oh.t[:], True, True, [rb.b, oh.b], [pb[0].b])
        CP("dve", vfar.t[:], pb[0].t[0:8, 0:1], [pb[0].b], [vfar.b])
        TS("dve", vrow.t[:], pb[0].t[0:8, 0:384], vfar.t[:, 0:1], None, ALU.subtract, None, [pb[0].b, vfar.b], [vrow.b])
        vrowd_b = Buf("vrowd")
        P.dma("sp", vrow_d, vrow.t[:], [vrow.b], [vrowd_b], ch_st)
        for di in range(2):
            base = 128 if di == 0 else 0
            src = bass.AP(tensor=vrow_d.tensor, offset=base, ap=[[1, 128], [384, 8], [1, 128]])
            P.dma("sp", xt.t[:], src, [vrowd_b], [xt.b], ch_ld[0])
            for h in range(8):
                MM(pb[1 + h // 4].t[:, (h % 4) * 128:(h % 4 + 1) * 128], xt.t[:, h, :], anti.t[:], True, True,
                   [xt.b, anti.b], [pb[1 + h // 4].b])
            for hh in range(2):
                CP("act", biasT[di].t[:, hh * 4:(hh + 1) * 4, :],
                   pb[1 + hh].t[:].rearrange("p (h t) -> p h t", h=4), [pb[1 + hh].b], [biasT[di].b])
        barrier()

    def ckpt(name):
        MARKS.append((name, sum(1 for o in P.ops if o[0] == "pe")))
        if stop == name:
            raise _Stop()

    def _layers():
        for l in range(DEPTH):
            xin_d = x_d if l == 0 else x2_d
            last = (l == DEPTH - 1)

            with contextlib.ExitStack() as L:
                NC1 = 4608
                w1 = sbt(L, "w1", [128, 8, NC1], BF16)
                wro = sbt(L, "wro", [128, 4, D], BF16)
                wmo = sbt(L, "wmo", [128, 4, D], BF16)
                wkv = sbt(L, "wkv", [128, 8, D], BF16)
                colmap = [(C_Q, 2048, 0), (C_MQ, 512, 2048), (C_G0, 1024, 2560), (C_G2, 1024, 3584)]
                for (src0, ncol, dst0) in colmap:
                    for s0 in range(0, ncol, 512):
                        P.dma("pool", w1.t[:, :, dst0 + s0:dst0 + s0 + 512],
                              w_in_d[l, :, src0 + s0:src0 + s0 + 512].rearrange("(c p) n -> p c n", p=128),
                              [], [w1.b], ch_w)
                P.dma("pool", wro.t[:], w_ret_o_d[l].rearrange("(c p) n -> p c n", p=128), [], [wro.b], ch_w)
                P.dma("pool", wmo.t[:], w_mem_o_d[l].rearrange("(c p) n -> p c n", p=128), [], [wmo.b], ch_w)
                for s0 in range(0, D, 512):
                    P.dma("pool", wkv.t[:, :, s0:s0 + 512],
                          w_mkv_d[l, :, s0:s0 + 512].rearrange("(c p) n -> p c n", p=128), [], [wkv.b], ch_w)
                dmask = sbt(L, "dmask", [128, 1024], F32)
                tabA = sbt(L, "tabA", [128, 512], F32)
                tabB = sbt(L, "tabB", [128, 512], F32)
                g64 = sbt(L, "g64", [128, 256], F32)
                kdec = sbt(L, "kdec", [128, 8], F32)
                for tt_, dd_ in ((dmask, dmask_d), (tabA, tabA_d), (tabB, tabB_d), (g64, g64_d), (kdec, kdec_d)):
                    P.dma("sp", tt_.t[:], dd_, [], [tt_.b], ch_c)
                xs = [sbt(L, "xs", [128, D], F32) for _ in range(2)]
                cs = [sbt(L, "cs", [128, 64], F32) for _ in range(2)]
                sn = [sbt(L, "sn", [128, 64], F32) for _ in range(2)]
                junk = sbt(L, "junk", [128, D], BF16)
                hn = sbt(L, "hn", [128, D], BF16)
                hT = sbt(L, "hT", [128, 8, 128], BF16)
                tA = sbt(L, "tA", [128, 512], F32)
                tBm = sbt(L, "tBm", [128, 512], F32)
                q_r = sbt(L, "q_r", [128, 512], BF16)
                k_r = sbt(L, "k_r", [128, 512], BF16)
                kdA = sbt(L, "kdA", [128, 512], BF16)
                kdB = sbt(L, "kdB", [128, 512], BF16)
                v_s = sbt(L, "v_s", [128, 512], BF16)
                sg = sbt(L, "sg", [128, 512], F32)
                qT = sbt(L, "qT", [128, 512], BF16)
                qA = sbt(L, "qA", [128, 512], BF16)
                qB = sbt(L, "qB", [128, 512], BF16)
                kTe = sbt(L, "kTe", [128, 512], BF16)
                kTo = sbt(L, "kTo", [128, 512], BF16)
                sT = sbt(L, "sT", [128, 1024], BF16)
                S32 = [sbt(L, "S32", [128, 256], F32) for _ in range(2)]
                S16 = [sbt(L, "S16", [128, 2, 256], BF16) for _ in range(2)]
                for z_ in (kdA, kdB, kTe, kTo, S16[0], S16[1]):
                    MEMSET("pool", z_.t[:], 0.0, [z_.b])
                stmp = sbt(L, "stmp", [128, 256], F32)
                o_sb = sbt(L, "o_sb", [128, 512], F32)
                o_sq = tA
                st8 = sbt(L, "st8", [128, 32], F32)
                yb = tBm
                ret = sbt(L, "ret", [128, 512], BF16)
                retT = qB
                mqT = qT
                pT = sT
                rs = tBm
                memoT = qA
                g0 = o_sb
                g2 = sg
                t1 = tA
                t2 = tBm
                part = sbt(L, "part", [128, D], F32)
                memT = sbt(L, "memT", [128, 8, 256], BF16)
                KT = sbt(L, "KT", [128, 4, 256], BF16)
                Vm = sbt(L, "Vm", [128, 2, 512], BF16)

                def norm_to_T(xtile, gT, gcol0, dstT, dst_cols, bank):
                    ACT(junk.t[:], xtile.t[:], AF.Square, [xtile.b], [junk.b, ss.b], accum_out=ss.t[:])
                    TS("dve", rstd.t[:], ss.t[:], 1.0 / D, 1e-6, ALU.mult, ALU.add, [ss.b], [rstd.b])
                    ACT(rstd.t[:], rstd.t[:], AF.Sqrt, [rstd.b], [rstd.b])
                    RECIP(rstd.t[:], rstd.t[:], [rstd.b], [rstd.b])
                    TS("dve", hn.t[:], xtile.t[:], rstd.t[:, 0:1], None, ALU.mult, None, [xtile.b, rstd.b], [hn.b])
                    for c in range(8):
                        TR(pbf(bank)[:, c * 128:(c + 1) * 128], hn.t[:, c * 128:(c + 1) * 128], [hn.b], [pb[bank].b])
                    TT("dve", dstT.t[:, :, dst_cols], pbf(bank).rearrange("p (c t) -> p c t", c=8),
                       gT.t[:, gcol0:gcol0 + 8].unsqueeze(2).to_broadcast([128, 8, 128]), ALU.mult,
                       [pb[bank].b, gT.b], [dstT.b])

                def p1_load(n):
                    sl = n % 2
                    t = n % NTS
                    P.dma("sp", xs[sl].t[:], xin_d[n * 128:(n + 1) * 128, :], [], [xs[sl].b], ch_ld[sl])
                    P.dma("sp", cs[sl].t[:], cos_d[t * 128:(t + 1) * 128, :], [], [cs[sl].b], ch_ld2[sl])
                    P.dma("sp", sn[sl].t[:], sin_d[t * 128:(t + 1) * 128, :], [], [sn[sl].b], ch_ld3[sl])

                def rope(bank, dst, sl):
                    v3 = lambda ap: ap.rearrange("p (h d) -> p h d", h=8)
                    cb = cs[sl].t[:].unsqueeze(1).to_broadcast([128, 8, 64])
                    sb_ = sn[sl].t[:].unsqueeze(1).to_broadcast([128, 8, 64])
                    TT("dve", v3(tA.t[:]), v3(pb[bank].t[:]), cb, ALU.mult, [pb[bank].b, cs[sl].b], [tA.b])
                    TT("dve", v3(tBm.t[:]), v3(pb[bank].t[:]), sb_, ALU.mult, [pb[bank].b, sn[sl].b], [tBm.b])
                    TT("pool", v3(dst.t[:])[:, :, 0:32], v3(tA.t[:])[:, :, 0:32], v3(tBm.t[:])[:, :, 32:64], ALU.subtract,
                       [tA.b, tBm.b], [dst.b])
                    TT("pool", v3(dst.t[:])[:, :, 32:64], v3(tA.t[:])[:, :, 32:64], v3(tBm.t[:])[:, :, 0:32], ALU.add,
                       [tA.b, tBm.b], [dst.b])

                def mem_prologue(seq):
                    for mc in range(2):
                        r0 = seq * 256 + mc * 128
                        P.dma("sp", xs[0].t[:], mem_d[r0:r0 + 128, :], [], [xs[0].b], ch_ld[0])
                        norm_to_T(xs[0], nmT, l * 8, memT, slice(mc * 128, (mc + 1) * 128), 0)
                    for h in range(4):
                        bank = 1 + h // 2
                        for c in range(8):
                            MM(pb[bank].t[:, (h % 2) * 256:(h % 2 + 1) * 256], w_kvs(c, h * 128, 128), memT.t[:, c, :],
                               c == 0, c == 7, [wkv.b, memT.b], [pb[bank].b])
                    for hh in range(2):
                        CP("act", KT.t[:, hh * 2:(hh + 1) * 2, :], pb[1 + hh].t[:].rearrange("p (h m) -> p h m", h=2),
                           [pb[1 + hh].b], [KT.b])
                    for mc in range(2):
                        for c in range(8):
                            MM(pb[3 + mc].t[:], memT.t[:, c, mc * 128:(mc + 1) * 128], w_kvs(c, 512, 512),
                               c == 0, c == 7, [wkv.b, memT.b], [pb[3 + mc].b])
                        CP("dve", Vm.t[:, mc, :], pb[3 + mc].t[:], [pb[3 + mc].b], [Vm.b])

                def w_kvs(c, c0, n):
                    return wkv.t[:, c, c0:c0 + n]

                def p1_tile(n):
                    sl = n % 2
                    seq, t = divmod(n, NTS)
                    if t == 0:
                        MEMSET("pool", S32[0].t[:], 0.0, [S32[0].b])
                        MEMSET("pool", S16[0].t[:], 0.0, [S16[0].b])
                    norm_to_T(xs[sl], n1T, l * 8, hT, slice(0, 128), 0)
                    P.dma("sp", hT_d[n * 128:(n + 1) * 128, :], hT.t[:].rearrange("p c t -> p (c t)"), [hT.b], [], ch_st)
                    for blk, bank in ((0, 1), (1, 2), (2, 3), (3, 4)):
                        for c in range(8):
                            MM(pb[bank].t[:], hT.t[:, c, :], w1.t[:, c, blk * 512:(blk + 1) * 512], c == 0, c == 7,
                               [hT.b, w1.b], [pb[bank].b])
                    rope(1, q_r, sl)
                    rope(2, k_r, sl)
                    CP("act", v_s.t[:], pb[3].t[:], [pb[3].b], [v_s.b])
                    ACT(sg.t[:], pb[4].t[:], AF.Silu, [pb[4].b], [sg.b])
                    for kd_, r0 in ((kdA, 0), (kdB, 64)):
                        TT("pool", kd_.t[r0:r0 + 64, :].rearrange("p (h d) -> p h d", h=8),
                           k_r.t[r0:r0 + 64, :].rearrange("p (h d) -> p h d", h=8),
                           kdec.t[r0:r0 + 64, :].unsqueeze(2).to_broadcast([64, 8, 64]), ALU.mult, [k_r.b, kdec.b], [kd_.b])
                    for c in range(4):
                        TR(pbf(0)[:, c * 128:(c + 1) * 128], q_r.t[:, c * 128:(c + 1) * 128], [q_r.b], [pb[0].b])
                    for c in range(4):
                        TR(pbf(0)[:, 512 + c * 128:512 + (c + 1) * 128], k_r.t[:, c * 128:(c + 1) * 128], [k_r.b], [pb[0].b])
                    CP("act", qT.t[:], pbf(0)[:, 0:512], [pb[0].b], [qT.b])
                    TT("dve", qA.t[:], qT.t[:], tabA.t[:], ALU.mult, [qT.b, tabA.b], [qA.b])
                    TT("pool", qB.t[:], qT.t[:], tabB.t[:], ALU.mult, [qT.b, tabB.b], [qB.b])
                    CP("act", kTe.t[0:64, :], pbf(0)[0:64, 512:1024], [pb[0].b], [kTe.b])
                    CP("act", kTo.t[64:128, :], pbf(0)[64:128, 512:1024], [pb[0].b], [kTo.b])
                    for h in range(8):
                        p_, b_ = h // 2, 64 * (h % 2)
                        bank = 1 + h // 4
                        kT_ = kTe if h % 2 == 0 else kTo
                        MM(pb[bank].t[:, (h % 4) * 128:(h % 4 + 1) * 128], kT_.t[:, p_ * 128:(p_ + 1) * 128],
                           qT.t[:, p_ * 128:(p_ + 1) * 128], True, True, [kT_.b, qT.b], [pb[bank].b])
                    for hh in range(2):
                        TT("dve", sT.t[:, hh * 512:(hh + 1) * 512], pb[1 + hh].t[:], dmask.t[:, hh * 512:(hh + 1) * 512],
                           ALU.mult, [pb[1 + hh].b, dmask.b], [sT.b])
                    for ci in range(2):
                        r0 = 64 * ci
                        bank = 3 + ci
                        kd_ = kdA if ci == 0 else kdB
                        for p_ in range(4):
                            MM(pb[bank].t[:, p_ * 128:(p_ + 1) * 128], kd_.t[:, p_ * 128:(p_ + 1) * 128],
                               v_s.t[:, p_ * 128:(p_ + 1) * 128], True, True, [kd_.b, v_s.b], [pb[bank].b])
                        src, dst = S32[ci], S32[1 - ci]
                        TT("dve", stmp.t[:], src.t[:], g64.t[:], ALU.mult, [src.b, g64.b], [stmp.b])
                        kv4 = pb[bank].t[:].rearrange("p (a e) -> p a e", a=4)
                        d3 = dst.t[:].rearrange("p (a e) -> p a e", a=4)
                        s3 = stmp.t[:].rearrange("p (a e) -> p a e", a=4)
                        TT("dve", d3[0:64], s3[0:64], kv4[0:64, :, 0:64], ALU.add, [stmp.b, pb[bank].b], [dst.b])
                        TT("dve", d3[64:128], s3[64:128], kv4[64:128, :, 64:128], ALU.add, [stmp.b, pb[bank].b], [dst.b])
                        if ci == 0:
                            CP("pool", S16[1].t[0:64, 0, :], S32[1].t[0:64, :], [S32[1].b], [S16[1].b])
                            CP("pool", S16[1].t[64:128, 1, :], S32[1].t[64:128, :], [S32[1].b], [S16[1].b])
                    for h in range(8):
                        p_, b_ = h // 2, 64 * (h % 2)
                        o_ap = pb[5].t[:, h * 64:(h + 1) * 64]
                        MM(o_ap, sT.t[:, h * 128:(h + 1) * 128], v_s.t[:, h * 64:(h + 1) * 64], True, False,
                           [sT.b, v_s.b], [pb[5].b])
                        MM(o_ap, qA.t[:, p_ * 128:(p_ + 1) * 128], S16[0].t[:, h % 2, p_ * 64:(p_ + 1) * 64],
                           False, False, [qA.b, S16[0].b], [pb[5].b])
                        MM(o_ap, qB.t[:, p_ * 128:(p_ + 1) * 128], S16[1].t[:, h % 2, p_ * 64:(p_ + 1) * 64],
                           False, True, [qB.b, S16[1].b], [pb[5].b])
                    CP("pool", S16[0].t[0:64, 0, :], S32[0].t[0:64, :], [S32[0].b], [S16[0].b])
                    CP("pool", S16[0].t[64:128, 1, :], S32[0].t[64:128, :], [S32[0].b], [S16[0].b])
                    CP("act", o_sb.t[:], pb[5].t[:], [pb[5].b], [o_sb.b])
                    ACT(o_sq.t[:], pb[5].t[:], AF.Square, [pb[5].b], [o_sq.b])
                    o3 = o_sb.t[:].rearrange("p (h e) -> p h e", h=8)
                    RED(st8.t[:, 0:8], o3, ALU.add, [o_sb.b], [st8.b])
                    RED(st8.t[:, 8:16], o_sq.t[:].rearrange("p (h e) -> p h e", h=8), ALU.add, [o_sq.b], [st8.b])
                    TS("dve", st8.t[:, 16:24], st8.t[:, 0:8], 1.0 / 64, None, ALU.mult, None, [st8.b], [st8.b])
                    TT("dve", st8.t[:, 0:8], st8.t[:, 16:24], st8.t[:, 16:24], ALU.mult, [st8.b], [st8.b])
                    STT(st8.t[:, 24:32], st8.t[:, 8:16], 1.0 / 64, st8.t[:, 0:8], ALU.mult, ALU.subtract, [st8.b], [st8.b])
                    TS("dve", st8.t[:, 24:32], st8.t[:, 24:32], 1e-5, None, ALU.add, None, [st8.b], [st8.b])
                    ACT(st8.t[:, 24:32], st8.t[:, 24:32], AF.Sqrt, [st8.b], [st8.b])
                    RECIP(st8.t[:, 24:32], st8.t[:, 24:32], [st8.b], [st8.b])
                    y3 = yb.t[:].rearrange("p (h e) -> p h e", h=8)
                    TT("dve", y3, o3, st8.t[:, 16:24].unsqueeze(2).to_broadcast([128, 8, 64]), ALU.subtract,
                       [o_sb.b, st8.b], [yb.b])
                    TT("pool", y3, y3, st8.t[:, 24:32].unsqueeze(2).to_broadcast([128, 8, 64]), ALU.mult,
                       [yb.b, st8.b], [yb.b])
                    TT("pool", ret.t[:], yb.t[:], sg.t[:], ALU.mult, [yb.b, sg.b], [ret.b])
                    for c in range(4):
                        TR(pbf(0)[:, c * 128:(c + 1) * 128], ret.t[:, c * 128:(c + 1) * 128], [ret.b], [pb[0].b])
                    CP("act", retT.t[:], pbf(0)[:, 0:512], [pb[0].b], [retT.b])
                    for j in range(4):
                        for c in range(8):
                            MM(pb[1].t[:, j * 128:(j + 1) * 128], w1.t[:, c, 2048 + j * 128:2048 + (j + 1) * 128],
                               hT.t[:, c, :], c == 0, c == 7, [w1.b, hT.b], [pb[1].b])
                    CP("dve", mqT.t[:], pb[1].t[:], [pb[1].b], [mqT.b])
                    for mc in range(2):
                        for h in range(4):
                            MM(pb[2 + mc].t[:, h * 128:(h + 1) * 128], KT.t[:, h, mc * 128:(mc + 1) * 128],
                               mqT.t[:, h * 128:(h + 1) * 128], True, True, [KT.b, mqT.b], [pb[2 + mc].b])
                        ACT(pT.t[:, mc * 512:(mc + 1) * 512], pb[2 + mc].t[:], AF.Exp, [pb[2 + mc].b], [pT.b],
                            scale=128.0 ** -0.5)
                    for h in range(4):
                        for mc in range(2):
                            MM(pb[4].t[:, h * 128:(h + 1) * 128], Vm.t[:, mc, h * 128:(h + 1) * 128],
                               pT.t[:, mc * 512 + h * 128:mc * 512 + (h + 1) * 128], mc == 0, mc == 1,
                               [Vm.b, pT.b], [pb[4].b])
                    for mc in range(2):
                        MM(pb[5].t[:], onesb.t[:], pT.t[:, mc * 512:(mc + 1) * 512], mc == 0, mc == 1,
                           [onesb.b, pT.b], [pb[5].b])
                    RECIP(rs.t[:], pb[5].t[:], [pb[5].b], [rs.b])
                    TT("dve", memoT.t[:], pb[4].t[:], rs.t[:], ALU.mult, [pb[4].b, rs.b], [memoT.b])
                    for blk in range(2):
                        cs_ = slice(blk * 512, (blk + 1) * 512)
                        for c in range(4):
                            MM(pb[6].t[:], retT.t[:, c * 128:(c + 1) * 128], wro.t[:, c, cs_], c == 0, c == 3,
                               [retT.b, wro.b], [pb[6].b])
                        for c in range(4):
                            MM(pb[7].t[:], memoT.t[:, c * 128:(c + 1) * 128], wmo.t[:, c, cs_], c == 0, c == 3,
                               [memoT.b, wmo.b], [pb[7].b])
                        for c in range(8):
                            MM(pb[2].t[:], hT.t[:, c, :], w1.t[:, c, 2560 + blk * 512:2560 + (blk + 1) * 512], c == 0, c == 7,
                               [hT.b, w1.b], [pb[2].b])
                        for c in range(8):
                            MM(pb[3].t[:], hT.t[:, c, :], w1.t[:, c, 3584 + blk * 512:3584 + (blk + 1) * 512], c == 0, c == 7,
                               [hT.b, w1.b], [pb[3].b])
                        ACT(g0.t[:], pb[2].t[:], AF.Sigmoid, [pb[2].b], [g0.b])
                        ACT(g2.t[:], pb[3].t[:], AF.Sigmoid, [pb[3].b], [g2.b])
                        TT("dve", t1.t[:], pb[6].t[:], g0.t[:], ALU.mult, [pb[6].b, g0.b], [t1.b])
                        TT("dve", t2.t[:], pb[7].t[:], g2.t[:], ALU.mult, [pb[7].b, g2.b], [t2.b])
                        TT("pool", part.t[:, cs_], t1.t[:], t2.t[:], ALU.add, [t1.b, t2.b], [part.b])
                    P.dma("sp", part_d[n * 128:(n + 1) * 128, :], part.t[:], [part.b], [], ch_st2)

                ckpt("p1w")
                for n in range(NT):
                    if n % NTS == 0:
                        mem_prologue(n // NTS)
                        ckpt("p1m")
                        p1_load(n)
                    if (n + 1) % NTS != 0:
                        p1_load(n + 1)
                    p1_tile(n)
                    ckpt("p1t")
                barrier()
                ckpt("p1_%d" % l)

            with contextlib.ExitStack() as L:
                NC2 = 424 + 1024
                w2 = sbt(L, "w2", [128, 8, NC2], BF16)
                wuq = sbt(L, "wuq", [128, 2, 512], BF16)
                wiq = sbt(L, "wiq", [128, 2, 256], BF16)
                wuk = sbt(L, "wuk", [128, 8, 128], BF16)
                wuv = sbt(L, "wuv", [128, 8 * 128], BF16)
                wdo = sbt(L, "wdo", [128, 4, D], BF16)
                wo = sbt(L, "wo", [128, 8, D], BF16)
                P.dma("pool", w2.t[:, :, 0:424], w_in_d[l, :, C_CQ:C_CQ + 424].rearrange("(c p) n -> p c n", p=128),
                      [], [w2.b], ch_w)
                for s0 in range(0, 1024, 512):
                    P.dma("pool", w2.t[:, :, 424 + s0:424 + s0 + 512],
                          w_in_d[l, :, C_G1 + s0:C_G1 + s0 + 512].rearrange("(c p) n -> p c n", p=128), [], [w2.b], ch_w)
                P.dma("pool", wuq.t[:], w_uq_d[l].rearrange("(c p) n -> p c n", p=128), [], [wuq.b], ch_w)
                P.dma("pool", wiq.t[:], w_iq_d[l].rearrange("(c p) n -> p c n", p=128), [], [wiq.b], ch_w)
                P.dma("pool", wuk.t[:], w_ukT_d[l].rearrange("(c p) n -> p c n", p=128), [], [wuk.b], ch_w)
                P.dma("pool", wuv.t[:], w_uvp_d[l], [], [wuv.b], ch_w)
                P.dma("pool", wdo.t[:], w_dsa_o_d[l].rearrange("(c p) n -> p c n", p=128), [], [wdo.b], ch_w)
                for s0 in range(0, D, 512):
                    P.dma("pool", wo.t[:, :, s0:s0 + 512],
                          w_out_d[l, :, s0:s0 + 512].rearrange("(c p) n -> p c n", p=128), [], [wo.b], ch_w)
                gkv = sbt(L, "gkv", [128, 128], F32)
                P.dma("sp", gkv.t[:], kvn_d[l:l + 1, :].to_broadcast([128, 128]), [], [gkv.b], ch_c)
                pw = sbt(L, "pw", [128, NIT + 2], F32)
                P.dma("sp", pw.t[:], pw_d, [], [pw.b], ch_c)
                hTs = [sbt(L, "hTs", [128, 8, 128], BF16) for _ in range(2)]
                pts = [sbt(L, "pts", [128, D], F32) for _ in range(2)]
                xs = [sbt(L, "xs2", [128, D], F32) for _ in range(2)]
                ckv = sbt(L, "ckv", [128, NTS, 128], BF16)
                ckvT = sbt(L, "ckvT", [128, S], BF16)
                kiT = sbt(L, "kiT", [32, S], BF16)
                acc = sbt(L, "acc", [128, S], F32)
                nm = sbt(L, "nm", [128, S], BF16)
                nmT_ = sbt(L, "nmT_", [128, NTS, 128], BF16)
                rt = [sbt(L, "rt", [128, 512], F32) for _ in range(2)]
                cqT = sbt(L, "cqT", [128, 2, 128], BF16)
                sq = sbt(L, "sq", [128, 2, 128], BF16)
                rq8 = sbt(L, "rq8", [128, 128], F32)
                qTd = sbt(L, "qTd", [128, 4, 128], BF16)
                qlat2 = [sbt(L, "qlat", [128, 8, 128], BF16) for _ in range(2)]
                qiT = sbt(L, "qiT", [32, 8, 128], BF16)
                iw = sbt(L, "iw", [128, 8], F32)
                jk = sbt(L, "jk", [128, 128], F32)
                g1s = [sbt(L, "g1", [128, D], F32) for _ in range(2)]
                th = sbt(L, "th", [128, 8], F32)
                mx8 = sbt(L, "mx8", [128, 8], F32)
                steps = sbt(L, "steps", [128, NIT + 2], F32)
                pTs = [sbt(L, "pTs", [128, 512], BF16) for _ in range(2)]
                rs2 = sbt(L, "rs2", [128, 512], F32)
                olat = sbt(L, "olat", [128, 8, 128], BF16)
                dsaT = sbt(L, "dsaT", [128, 4, 128], BF16)
                m1 = sbt(L, "m1", [128, D], F32)
                mg = sbt(L, "mg", [128, D], BF16)
                mT = sbt(L, "mT", [128, 8, 128], BF16)
                x1 = sbt(L, "x1", [128, D], F32)

                def p2_load_h(n):
                    sl = n % 2
                    P.dma("sp", hTs[sl].t[:].rearrange("p c t -> p (c t)"), hT_d[n * 128:(n + 1) * 128, :], [],
                          [hTs[sl].b], ch_ld[sl])

                def p2_load_px(n):
                    sl = n % 2
                    P.dma("sp", pts[sl].t[:], part_d[n * 128:(n + 1) * 128, :], [], [pts[sl].b], ch_ld2[sl])
                    P.dma("sp", xs[sl].t[:], xin_d[n * 128:(n + 1) * 128, :], [], [xs[sl].b], ch_ld3[sl])

                def weave(*gens):
                    gens = list(gens)
                    while gens:
                        for g in list(gens):
                            try:
                                next(g)
                            except StopIteration:
                                gens.remove(g)

                def p2_part(n, part):
                    sl = n % 2
                    seq, t = divmod(n, NTS)
                    hT = hTs[sl]
                    NK = 128 * (t + 1)
                    qlat = qlat2[sl]
                    g1 = g1s[sl]
                    if part == "A":
                        for j in range(2):
                            for c in range(8):
                                MM(pb[7].t[:, j * 128:(j + 1) * 128], w2.t[:, c, j * 128:(j + 1) * 128], hT.t[:, c, :],
                                   c == 0, c == 7, [w2.b, hT.b], [pb[7].b])
                        for j in range(2):
                            TS("dve", cqT.t[:, j, :], pb[7].t[:, j * 128:(j + 1) * 128], qnT.t[:, l * 2 + j:l * 2 + j + 1], None,
                               ALU.mult, None, [pb[7].b, qnT.b], [cqT.b])
                        ACT(sq.t[:].rearrange("p a t -> p (a t)"), pb[7].t[:, 0:256], AF.Square, [pb[7].b], [sq.b])
                        for j in range(2):
                            MM(pb[6].t[:, 0:128], onesb.t[:], sq.t[:, j, :], j == 0, j == 1, [onesb.b, sq.b], [pb[6].b])
                        TS("dve", rq8.t[:], pb[6].t[:, 0:128], 0.25, 64e-6, ALU.mult, ALU.add, [pb[6].b], [rq8.b])
                        ACT(rq8.t[:], rq8.t[:], AF.Sqrt, [rq8.b], [rq8.b])
                        RECIP(rq8.t[:], rq8.t[:], [rq8.b], [rq8.b])
                        for j in range(4):
                            for rc in range(2):
                                MM(pb[5].t[:, j * 128:(j + 1) * 128], wuq.t[:, rc, j * 128:(j + 1) * 128], cqT.t[:, rc, :],
                                   rc == 0, rc == 1, [wuq.b, cqT.b], [pb[5].b])
                        CP("act", qTd.t[:].rearrange("p a t -> p (a t)"), pb[5].t[:], [pb[5].b], [qTd.b])
                        for h in range(8):
                            p_, b_ = h // 2, 64 * (h % 2)
                            bank = 2 + h // 4
                            MM(pb[bank].t[:, (h % 4) * 128:(h % 4 + 1) * 128], wuk.t[:, (h % 2) * 4 + p_, :],
                               qTd.t[:, p_, :], True, True, [wuk.b, qTd.b], [pb[bank].b])
                        for hh in range(2):
                            TT("dve", qlat.t[:, hh * 4:(hh + 1) * 4, :], pb[2 + hh].t[:].rearrange("p (h t) -> p h t", h=4),
                               rq8.t[:].unsqueeze(1).to_broadcast([128, 4, 128]), ALU.mult, [pb[2 + hh].b, rq8.b], [qlat.b])
                        for h in range(8):
                            bank = (4, 6)[h // 4]
                            for rc in range(2):
                                MM(pb[bank].t[0:32, (h % 4) * 128:(h % 4 + 1) * 128], wiq.t[:, rc, h * 32:(h + 1) * 32],
                                   cqT.t[:, rc, :], rc == 0, rc == 1, [wiq.b, cqT.b], [pb[bank].b])
                        for hh in range(2):
                            CP("act", qiT.t[:, hh * 4:(hh + 1) * 4, :], pb[(4, 6)[hh]].t[0:32, :].rearrange("p (h t) -> p h t", h=4),
                               [pb[(4, 6)[hh]].b], [qiT.b])
                        for c in range(8):
                            MM(pb[1].t[:, 0:128], hT.t[:, c, :], w2.t[:, c, 256:384], c == 0, c == 7, [hT.b, w2.b], [pb[1].b])
                        ACT(jk.t[:], pb[1].t[:, 0:128], AF.Square, [pb[1].b], [jk.b, ss.b], accum_out=ss.t[:])
                        TS("dve", rstd.t[:], ss.t[:], 1.0 / 128, 1e-6, ALU.mult, ALU.add, [ss.b], [rstd.b])
                        ACT(rstd.t[:], rstd.t[:], AF.Sqrt, [rstd.b], [rstd.b])
                        RECIP(rstd.t[:], rstd.t[:], [rstd.b], [rstd.b])
                        STT(ckv.t[:, t, :], pb[1].t[:, 0:128], rstd.t[:, 0:1], gkv.t[:], ALU.mult, ALU.mult,
                            [pb[1].b, rstd.b, gkv.b], [ckv.b])
                        TR(pbf(0)[:, 0:128], ckv.t[:, t, :], [ckv.b], [pb[0].b])
                        CP("act", ckvT.t[:, t * 128:(t + 1) * 128], pbf(0)[:, 0:128], [pb[0].b], [ckvT.b])
                        for c in range(8):
                            MM(pb[1].t[0:32, 128:256], w2.t[:, c, 384:416], hT.t[:, c, :], c == 0, c == 7, [w2.b, hT.b], [pb[1].b])
                        for c in range(8):
                            MM(pb[1].t[:, 256:264], hT.t[:, c, :], w2.t[:, c, 416:424], c == 0, c == 7, [hT.b, w2.b], [pb[1].b])
                        CP("act", kiT.t[:, t * 128:(t + 1) * 128], pb[1].t[0:32, 128:256], [pb[1].b], [kiT.b])
                        CP("dve", iw.t[:], pb[1].t[:, 256:264], [pb[1].b], [iw.b])
                        for blk in range(2):
                            for c in range(8):
                                MM(pb[6 + blk].t[:], hT.t[:, c, :], w2.t[:, c, 424 + blk * 512:424 + (blk + 1) * 512], c == 0, c == 7,
                                   [hT.b, w2.b], [pb[6 + blk].b])
                            ACT(g1.t[:, blk * 512:(blk + 1) * 512], pb[6 + blk].t[:], AF.Sigmoid, [pb[6 + blk].b], [g1.b])
                    if part == "A2":
                        kb = 0
                        ctr = 0
                        while kb < NK:
                            wk = min(512, NK - kb)
                            for h in range(8):
                                r_ = ctr % 2
                                bank = 6 + r_
                                MM(pb[bank].t[:, 0:wk], qiT.t[:, h, :], kiT.t[:, kb:kb + wk], True, True, [qiT.b, kiT.b],
                                   [pb[bank].b])
                                ACT(rt[r_].t[:, 0:wk], pb[bank].t[:, 0:wk], AF.Relu, [pb[bank].b], [rt[r_].b])
                                if h == 0:
                                    TS("dve", acc.t[:, kb:kb + wk], rt[r_].t[:, 0:wk], iw.t[:, 0:1], None, ALU.mult, None,
                                       [rt[r_].b, iw.b], [acc.b])
                                else:
                                    STT(acc.t[:, kb:kb + wk], rt[r_].t[:, 0:wk], iw.t[:, h:h + 1], acc.t[:, kb:kb + wk],
                                        ALU.mult, ALU.add, [rt[r_].b, iw.b, acc.b], [acc.b])
                                ctr += 1
                                yield
                            kb += wk
                    if part == "Bd":
                        if NK > KSEL:
                            MAX8(mx8.t[:], acc.t[:, 0:NK - 64], [acc.b], [mx8.b])
                            RED(th.t[:, 1:2], acc.t[:, 0:NK - 64], ALU.min, [acc.b], [th.b])
                            MAX8(mx8.t[64:128, :], acc.t[64:128, 0:NK], [acc.b], [mx8.b])
                            MEMSET("dve", acc.t[0:64, NK - 64:NK], -1e30, [acc.b])
                            TT("dve", th.t[:, 0:1], mx8.t[:, 0:1], th.t[:, 1:2], ALU.add, [mx8.b, th.b], [th.b])
                            TS("dve", th.t[:, 0:1], th.t[:, 0:1], 0.5, None, ALU.mult, None, [th.b], [th.b])
                            TT("dve", th.t[:, 2:3], mx8.t[:, 0:1], th.t[:, 1:2], ALU.subtract, [mx8.b, th.b], [th.b])
                            TS("dve", th.t[:, 2:3], th.t[:, 2:3], 0.5000001, 1e-30, ALU.mult, ALU.add, [th.b], [th.b])
                            TS("dve", steps.t[:], pw.t[:], th.t[:, 2:3], None, ALU.mult, None, [pw.b, th.b], [steps.b])
                            for k in range(NIT):
                                TS("dve", nm.t[:, 0:NK], acc.t[:, 0:NK], th.t[:, 0:1], 0.0, ALU.is_ge, ALU.add,
                                   [acc.b, th.b], [nm.b, th.b], accum_out=th.t[:, 3:4])
                                TS("dve", th.t[:, 4:5], th.t[:, 3:4], float(KSEL), 0.5, ALU.is_ge, ALU.subtract, [th.b], [th.b])
                                STT(th.t[:, 0:1], th.t[:, 4:5], steps.t[:, k:k + 1], th.t[:, 0:1], ALU.mult, ALU.add,
                                    [th.b, steps.b], [th.b])
                            TT("dve", th.t[:, 5:6], th.t[:, 0:1], steps.t[:, NIT:NIT + 1], ALU.subtract, [th.b, steps.b], [th.b])
                        else:
                            MEMSET("dve", acc.t[0:64, NK - 64:NK], -1e30, [acc.b])
                            MEMSET("dve", th.t[:, 5:6], -1e29, [th.b])
                        TS("dve", nm.t[:, 0:NK], acc.t[:, 0:NK], th.t[:, 5:6], NEG, ALU.is_lt, ALU.mult, [acc.b, th.b], [nm.b])
                    if part == "Bp":
                        for kt0 in range(0, t + 1, 8):
                            nblk = min(8, t + 1 - kt0)
                            for i in range(nblk):
                                kt = kt0 + i
                                TR(pbf(7)[:, i * 128:(i + 1) * 128], nm.t[:, kt * 128:(kt + 1) * 128], [nm.b], [pb[7].b])
                            CP("act", nmT_.t[:, kt0:kt0 + nblk, :].rearrange("p a t -> p (a t)"), pbf(7)[:, 0:nblk * 128],
                               [pb[7].b], [nmT_.b])
                    if part == "Ca":
                        ctr = 0
                        for kt in range(t + 1):
                            for half in range(2):
                                bank = half
                                sl2 = ctr % 2
                                MM(pb[bank].t[:], ckvT.t[:, kt * 128:(kt + 1) * 128],
                                   qlat.t[:, half * 4:(half + 1) * 4, :].rearrange("p h t -> p (h t)"), True, False,
                                   [ckvT.b, qlat.b], [pb[bank].b])
                                near = kt >= t - 1
                                MM(pb[bank].t[:].rearrange("p (h t) -> p h t", h=4), identb.t[:],
                                   nmT_.t[:, kt, :].unsqueeze(1).to_broadcast([128, 4, 128]), False, not near,
                                   [identb.b, nmT_.b], [pb[bank].b])
                                if near:
                                    MM(pb[bank].t[:].rearrange("p (h t) -> p h t", h=4), identb.t[:],
                                       biasT[t - kt].t[:, half * 4:(half + 1) * 4, :], False, True, [identb.b, biasT[t - kt].b],
                                       [pb[bank].b])
                                ACT(pTs[sl2].t[:], pb[bank].t[:], AF.Exp, [pb[bank].b], [pTs[sl2].b])
                                MM(pb[2 + half].t[:], ckv.t[:, kt, :], pTs[sl2].t[:], kt == 0, kt == t, [ckv.b, pTs[sl2].b],
                                   [pb[2 + half].b])
                                MM(pb[4 + half].t[:], onesb.t[:], pTs[sl2].t[:], kt == 0, kt == t, [onesb.b, pTs[sl2].b],
                                   [pb[4 + half].b])
                                ctr += 1
                                yield
                    if part == "Ct":
                        for half in range(2):
                            RECIP(rs2.t[:], pb[4 + half].t[:], [pb[4 + half].b], [rs2.b])
                            TT("dve", olat.t[:, half * 4:(half + 1) * 4, :].rearrange("p h t -> p (h t)"), pb[2 + half].t[:],
                               rs2.t[:], ALU.mult, [pb[2 + half].b, rs2.b], [olat.b])
                        for p_ in range(4):
                            for hi in range(2):
                                h = 2 * p_ + hi
                                MM(pb[6].t[:, p_ * 128:(p_ + 1) * 128], wuv.t[:, h * 128:(h + 1) * 128], olat.t[:, h, :],
                                   hi == 0, hi == 1, [wuv.b, olat.b], [pb[6].b])
                        CP("act", dsaT.t[:].rearrange("p a t -> p (a t)"), pb[6].t[:], [pb[6].b], [dsaT.b])
                        for blk in range(2):
                            cs_ = slice(blk * 512, (blk + 1) * 512)
                            for c in range(4):
                                MM(pb[6].t[:], dsaT.t[:, c, :], wdo.t[:, c, cs_], c == 0, c == 3, [dsaT.b, wdo.b], [pb[6].b])
                            TT("dve", m1.t[:, cs_], pb[6].t[:], g1.t[:, cs_], ALU.mult, [pb[6].b, g1.b], [m1.b])
                        TT("pool", mg.t[:], m1.t[:], pts[sl].t[:], ALU.add, [m1.b, pts[sl].b], [mg.b])
                        for c in range(8):
                            TR(pbf(7)[:, c * 128:(c + 1) * 128], mg.t[:, c * 128:(c + 1) * 128], [mg.b], [pb[7].b])
                        CP("act", mT.t[:].rearrange("p c t -> p (c t)"), pbf(7)[:, :], [pb[7].b], [mT.b])
                        for blk in range(2):
                            cs_ = slice(blk * 512, (blk + 1) * 512)
                            for c in range(8):
                                MM(pb[6].t[:], mT.t[:, c, :], wo.t[:, c, cs_], c == 0, c == 7, [mT.b, wo.b], [pb[6].b])
                            TT("dve", x1.t[:, cs_], pb[6].t[:], xs[sl].t[:, cs_], ALU.add, [pb[6].b, xs[sl].b], [x1.b])
                        P.dma("sp", x1_d[n * 128:(n + 1) * 128, :], x1.t[:], [x1.b], [], ch_st)

                ckpt("p2w")
                for seq_ in range(NSEQ):
                    n0_ = seq_ * NTS
                    p2_load_h(n0_)
                    for t_ in range(NTS):
                        n = n0_ + t_
                        if t_ + 1 < NTS:
                            p2_load_h(n + 1)
                        if t_ > 0:
                            p2_load_px(n - 1)
                        weave(p2_part(n, "A"))
                        gA2 = p2_part(n, "A2")
                        gCa = p2_part(n - 1, "Ca") if t_ > 0 else iter(())
                        k_ = 0
                        for _ in gA2:
                            k_ += 1
                            if k_ % 2 == 0:
                                next(gCa, None)
                        weave(p2_part(n, "Bd"))
                        weave(gCa)
                        weave(p2_part(n, "Bp"))
                        if t_ > 0:
                            weave(p2_part(n - 1, "Ct"))
                        ckpt("p2t%d" % n)
                    last_ = n0_ + NTS - 1
                    p2_load_px(last_)
                    weave(p2_part(last_, "Ca"))
                    weave(p2_part(last_, "Ct"))
                barrier()
                ckpt("p2_%d" % l)

            with contextlib.ExitStack() as L:
                wf1 = sbt(L, "wf1", [128, 8, 4096], BF16)
                wf2 = sbt(L, "wf2", [128, 32, D], BF16)
                for s0 in range(0, 4096, 512):
                    P.dma("pool", wf1.t[:, :, s0:s0 + 512],
                          w_ff1_d[l, :, s0:s0 + 512].rearrange("(c p) n -> p c n", p=128), [], [wf1.b], ch_w)
                for c0 in range(0, 32, 4):
                    P.dma("pool", wf2.t[:, c0:c0 + 4, :],
                          w_ff2_d[l, c0 * 128:(c0 + 4) * 128, :].rearrange("(c p) n -> p c n", p=128), [], [wf2.b], ch_w)
                xs = [sbt(L, "xs4", [128, D], F32) for _ in range(2)]
                junk = sbt(L, "junk4", [128, D], BF16)
                hn = sbt(L, "hn4", [128, D], BF16)
                h2T = sbt(L, "h2T", [128, 8, 128], BF16)
                rl = [sbt(L, "rl", [128, 512], F32) for _ in range(2)]
                uT = sbt(L, "uT", [128, 32, 128], BF16)
                x2 = sbt(L, "x2", [128, D], F32)
                fo = sbt(L, "fo", [128, D], F32)
                if last:
                    gfin = sbt(L, "gfin", [128, D], F32)
                    P.dma("sp", gfin.t[:], fin_d.to_broadcast([128, D]), [], [gfin.b], ch_c)

                def p4_load(n):
                    sl = n % 2
                    P.dma("sp", xs[sl].t[:], x1_d[n * 128:(n + 1) * 128, :], [], [xs[sl].b], ch_ld[sl])

                def p4_tile(n):
                    sl = n % 2
                    xt_ = xs[sl]
                    ACT(junk.t[:], xt_.t[:], AF.Square, [xt_.b], [junk.b, ss.b], accum_out=ss.t[:])
                    TS("dve", rstd.t[:], ss.t[:], 1.0 / D, 1e-6, ALU.mult, ALU.add, [ss.b], [rstd.b])
                    ACT(rstd.t[:], rstd.t[:], AF.Sqrt, [rstd.b], [rstd.b])
                    RECIP(rstd.t[:], rstd.t[:], [rstd.b], [rstd.b])
                    TS("dve", hn.t[:], xt_.t[:], rstd.t[:, 0:1], None, ALU.mult, None, [xt_.b, rstd.b], [hn.b])
                    for c in range(8):
                        TR(pbf(0)[:, c * 128:(c + 1) * 128], hn.t[:, c * 128:(c + 1) * 128], [hn.b], [pb[0].b])
                    TT("dve", h2T.t[:], pbf(0).rearrange("p (c t) -> p c t", c=8),
                       n2T.t[:, l * 8:l * 8 + 8].unsqueeze(2).to_broadcast([128, 8, 128]), ALU.mult, [pb[0].b, n2T.b],
                       [h2T.b])
                    for fg in range(8):
                        bank = 1 + fg % 4
                        for j in range(4):
                            fc = fg * 4 + j
                            for c in range(8):
                                MM(pb[bank].t[:, j * 128:(j + 1) * 128], wf1.t[:, c, fc * 128:(fc + 1) * 128], h2T.t[:, c, :],
                                   c == 0, c == 7, [wf1.b, h2T.b], [pb[bank].b])
                        r_ = rl[fg % 2]
                        ACT(r_.t[:], pb[bank].t[:], AF.Relu, [pb[bank].b], [r_.b])
                        TT("dve" if fg % 2 == 0 else "pool", uT.t[:, fg * 4:(fg + 1) * 4, :].rearrange("p a t -> p (a t)"),
                           r_.t[:], r_.t[:], ALU.mult, [r_.b], [uT.b])
                    for blk in range(2):
                        cs_ = slice(blk * 512, (blk + 1) * 512)
                        bank = 5 + blk
                        for fc in range(32):
                            MM(pb[bank].t[:], uT.t[:, fc, :], wf2.t[:, fc, cs_], fc == 0, fc == 31, [uT.b, wf2.b],
                               [pb[bank].b])
                        TT("dve", x2.t[:, cs_], pb[bank].t[:], xt_.t[:, cs_], ALU.add, [pb[bank].b, xt_.b], [x2.b])
                    if not last:
                        P.dma("sp", x2_d[n * 128:(n + 1) * 128, :], x2.t[:], [x2.b], [], ch_st)
                    else:
                        if dbg:
                            P.dma("sp", x2_d[n * 128:(n + 1) * 128, :], x2.t[:], [x2.b], [], ch_st)
                        ACT(junk.t[:], x2.t[:], AF.Square, [x2.b], [junk.b, ss.b], accum_out=ss.t[:])
                        TS("dve", rstd.t[:], ss.t[:], 1.0 / D, 1e-6, ALU.mult, ALU.add, [ss.b], [rstd.b])
                        ACT(rstd.t[:], rstd.t[:], AF.Sqrt, [rstd.b], [rstd.b])
                        RECIP(rstd.t[:], rstd.t[:], [rstd.b], [rstd.b])
                        STT(fo.t[:], x2.t[:], rstd.t[:, 0:1], gfin.t[:], ALU.mult, ALU.mult, [x2.b, rstd.b, gfin.b], [fo.b])
                        P.dma("sp", out_d[n * 128:(n + 1) * 128, :], fo.t[:], [fo.b], [], ch_out)

                p4_load(0)
                for n in range(NT):
                    if n + 1 < NT:
                        p4_load(n + 1)
                    p4_tile(n)
                barrier()
                ckpt("p4_%d" % l)


    try:
        ckpt("bias")
        _layers()
    except _Stop:
        pass
    stats = P.emit(final_chans=[ch_out, ch_st, ch_st2])
    return nc, es, stats


def prep_shared(inputs, S, DEPTH, NIT):
    f = lambda a: np.ascontiguousarray(np.asarray(a, dtype=np.float32))
    sh = {}
    for k in ("w_in", "w_ret_o", "w_dsa_o", "w_mem_o", "w_out", "w_mem_kv", "w_ff1", "w_ff2", "rel_bias", "kv_norm"):
        sh[k] = f(inputs[k])
    sh["w_uq"] = f(np.asarray(inputs["w_uq"]).reshape(DEPTH, 256, 512))
    sh["w_iq"] = f(np.asarray(inputs["w_iq"]).reshape(DEPTH, 256, 256))
    wuk = np.asarray(inputs["w_uk"], dtype=np.float32)
    wukT = wuk.reshape(DEPTH, 128, 512).transpose(0, 2, 1)
    eo = np.zeros((DEPTH, 2, 4, 128, 128), np.float32)
    w5 = wukT.reshape(DEPTH, 4, 2, 64, 128)
    eo[:, 0, :, 0:64, :] = w5[:, :, 0]
    eo[:, 1, :, 64:128, :] = w5[:, :, 1]
    sh["w_ukT"] = f(eo.reshape(DEPTH, 1024, 128))
    wuv = np.asarray(inputs["w_uv"], dtype=np.float32)
    pad = np.zeros((DEPTH, 128, 8, 128), np.float32)
    for h in range(8):
        pad[:, :, h, (h % 2) * 64:(h % 2) * 64 + 64] = wuv[:, :, h, :]
    sh["w_uvp"] = f(pad.reshape(DEPTH, 128, 1024))

    def colT(a, nch):
        a = np.asarray(a, dtype=np.float32).reshape(DEPTH, nch, 128)
        return f(a.transpose(2, 0, 1).reshape(128, DEPTH * nch))
    sh["norm1T"] = colT(inputs["norm1"], 8)
    sh["norm2T"] = colT(inputs["norm2"], 8)
    sh["mem_normT"] = colT(inputs["mem_norm"], 8)
    sh["q_normT"] = colT(inputs["q_norm"], 2)
    sh["final_norm"] = f(np.asarray(inputs["final_norm"]).reshape(1, D))
    c = make_consts(S, NIT)
    sh["ident"] = c["ident"]
    sh["antiI"] = c["antiI"]
    sh["cos2"] = c["cos2"]
    sh["sin2"] = c["sin2"]
    sh["dmaskT"] = f(c["dmaskT"].reshape(128, 1024))
    sh["tabA"] = f(c["tabA"].reshape(128, 512))
    sh["tabB"] = f(c["tabB"].reshape(128, 512))
    sh["g64"] = f(c["g64"].reshape(128, 256))
    sh["kdec"] = c["kdec"]
    sh["oh"] = c["oh"]
    sh["pw"] = c["pw"]
    return sh


_CACHE = {}


def run(inputs, ncores, NSEQ, S, DEPTH, NIT=16, dbg=False, stop=None):
    key = (S, NSEQ, DEPTH, NIT, dbg, stop)
    if key not in _CACHE:
        _CACHE[key] = build_program(S, NSEQ, DEPTH, NIT, dbg, stop)
    nc, es, stats = _CACHE[key]
    sh = prep_shared(inputs, S, DEPTH, NIT)
    x = np.asarray(inputs["x"], dtype=np.float32)
    mem = np.asarray(inputs["mem"], dtype=np.float32)
    in_maps = []
    for c in range(ncores):
        m = dict(sh)
        m["x"] = np.ascontiguousarray(x[c * NSEQ:(c + 1) * NSEQ].reshape(NSEQ * S, D))
        m["mem"] = np.ascontiguousarray(mem[c * NSEQ:(c + 1) * NSEQ].reshape(NSEQ * 256, D))
        in_maps.append(m)
    res = run_bass_kernel_spmd(nc, in_maps, core_ids=list(range(ncores)))
    return res


def kernel(**inputs):
    B, S, _ = inputs["x"].shape
    NSEQ = B // NCORES
    res = run(inputs, NCORES, NSEQ, S, 2)
    outs = [r["out"].reshape(NSEQ, S, D) for r in res.results]
    return np.concatenate(outs, axis=0).astype(np.float32)
```

```python
import contextlib
import math
import numpy as np
import concourse.bass as bass
import concourse.mybir as mybir
from concourse.bass_utils import run_bass_kernel_spmd

F32 = mybir.dt.float32
BF16 = mybir.dt.bfloat16
AF = mybir.ActivationFunctionType
ALU = mybir.AluOpType
AX = mybir.AxisListType

D = 1024
N_IN = 6056
NCORES = 8
C_Q, C_K, C_V, C_G, C_CQ, C_CKV, C_IK, C_IW, C_MQ, C_G0, C_G1, C_G2 = (
    0, 512, 1024, 1536, 2048, 2304, 2432, 2464, 2472, 2984, 4008, 5032)
NEG = -30000.0


class Buf:
    __slots__ = ("name", "w", "r", "psum")

    def __init__(self, name, psum=False):
        self.name = name
        self.w = {}
        self.r = {}
        self.psum = psum


class TB:
    __slots__ = ("t", "b")

    def __init__(self, t, b):
        self.t = t
        self.b = b


class Prog:
    def __init__(self, nc, es):
        self.nc = nc
        self.es = es
        self.ops = []
        self.engs = ("pe", "act", "dve", "pool", "sp")
        self.nchan = 0
        self.phase = Buf("PHASE")

    def chan(self):
        self.nchan += 1
        return self.nchan - 1

    maxops = None

    def op(self, eng, fn, reads=(), writes=(), chan=None, mm=False, barrier=False, extra=()):
        i = len(self.ops)
        if Prog.maxops is not None and i >= Prog.maxops:
            return i - 1
        deps = set(extra)
        reads = list(reads)
        writes = list(writes)
        if barrier:
            writes.append(self.phase)
        else:
            reads.append(self.phase)
        key = eng if chan is None else ("c", chan)
        for b in reads:
            deps.update(b.w.values())
            if b.psum:
                deps.update(v for k, v in b.r.items() if k != key)
        for b in writes:
            deps.update(b.w.values())
            deps.update(b.r.values())
        if mm:
            deps = {d for d in deps if not (self.ops[d][4] and self.ops[d][0] == "pe")}
        self.ops.append([eng, fn, deps, chan, mm])
        for b in reads:
            b.r[key] = i
        for b in writes:
            b.w = {key: i}
            b.r = {}
        return i

    def dma(self, eng, out, in_, reads, writes, chan=None):
        rings = self.__dict__.setdefault("_rings", {})
        if eng not in rings:
            k = 8 if eng == "sp" else 3
            rings[eng] = [[self.chan() for _ in range(k)], 0, {}]
        ring = rings[eng]
        c = ring[0][ring[1] % len(ring[0])]
        ring[1] += 1
        extra = (ring[2][c],) if c in ring[2] else ()
        i = self.op(eng, lambda e: e.dma_start(out=out, in_=in_), reads, writes, chan=c, extra=extra)
        ring[2][c] = i
        return i

    def emit(self, final_chans=()):
        nc = self.nc
        ops = self.ops
        n = len(ops)
        has_dep = [False] * n
        for o in ops:
            for d in o[2]:
                has_dep[d] = True
        tick = [None] * n
        ecount = {e: 0 for e in self.engs}
        ccount = [0] * self.nchan
        for i, o in enumerate(ops):
            if o[3] is not None:
                ccount[o[3]] += 16
                tick[i] = (("c", o[3]), ccount[o[3]])
            elif has_dep[i]:
                ecount[o[0]] += 1
                tick[i] = (o[0], ecount[o[0]])
        sems = {}
        for e in self.engs:
            sems[e] = self.es.enter_context(nc.semaphore("s_" + e))
        for c in range(self.nchan):
            sems[("c", c)] = self.es.enter_context(nc.semaphore("c_%d" % c))
        per_eng = {e: [] for e in self.engs}
        for i, o in enumerate(ops):
            per_eng[o[0]].append(i)
        block = self.es.enter_context(nc.Block())
        final = [(("c", c), ccount[c]) for c in range(self.nchan) if ccount[c] > 0]

        def make(ename):
            def body(eobj):
                seen = {}
                for i in per_eng[ename]:
                    o = ops[i]
                    need = {}
                    for d in o[2]:
                        k, v = tick[d]
                        if need.get(k, 0) < v:
                            need[k] = v
                    for k, v in need.items():
                        if seen.get(k, 0) < v:
                            eobj.wait_ge(sems[k], v)
                            seen[k] = v
                    ins = o[1](eobj)
                    if tick[i] is not None:
                        k, v = tick[i]
                        ins.then_inc(sems[k], 16 if o[3] is not None else 1)
                if ename == "sp":
                    for k, v in final:
                        eobj.wait_ge(sems[k], v)
            return body

        block.tensor(make("pe"))
        block.scalar(make("act"))
        block.vector(make("dve"))
        block.gpsimd(make("pool"))
        block.sync(make("sp"))
        return {e: len(per_eng[e]) for e in per_eng}


def _t5_bucket(rel):
    nb = 16
    max_exact = 8
    base = np.where(rel > 0, nb, 0)
    n = np.abs(rel)
    nf = np.maximum(n, 1).astype(np.float32)
    large = max_exact + (np.log(nf / np.float32(max_exact)) / np.float32(math.log(128 / max_exact))
                         * np.float32(nb - max_exact)).astype(np.int32)
    large = np.minimum(large, nb - 1)
    return base + np.where(n < max_exact, n, large)


def make_consts(S, NIT):
    c = {}
    c["ident"] = np.eye(128, dtype=np.float32)
    c["antiI"] = np.ascontiguousarray(np.eye(128, dtype=np.float32)[::-1])
    pos = np.arange(S, dtype=np.float32)
    half = 32
    freqs = (np.float32(10000.0) ** (-np.arange(half, dtype=np.float32) / np.float32(half))).astype(np.float32)
    ang = pos[:, None] * freqs[None, :]
    cos = np.cos(ang).astype(np.float32)
    sin = np.sin(ang).astype(np.float32)
    c["cos2"] = np.concatenate([cos, cos], 1)
    c["sin2"] = np.concatenate([sin, sin], 1)
    H = 8
    log_g = np.log1p(-np.exp2(-5.0 - np.arange(H, dtype=np.float64)))
    i = np.arange(128)
    same = (i[:, None] // 64) == (i[None, :] // 64)
    dm = np.zeros((128, H, 128), np.float64)
    for h in range(H):
        dm[:, h, :] = np.where(same, np.exp(log_g[h] * np.abs(i[:, None] - i[None, :])), 0.0) * 0.125
    c["dmaskT"] = dm.astype(np.float32)
    tabA = np.zeros((128, 4, 128), np.float64)
    tabB = np.zeros((128, 4, 128), np.float64)
    g64 = np.zeros((128, 4, 64), np.float64)
    for p in range(4):
        for half_i in range(2):
            h = 2 * p + half_i
            rows = slice(64 * half_i, 64 * half_i + 64)
            qd = np.exp(log_g[h] * (np.arange(64) + 1))
            tabA[rows, p, 0:64] = qd[None, :]
            tabB[rows, p, 64:128] = qd[None, :]
            g64[rows, p, :] = np.exp(log_g[h] * 64)
    c["tabA"] = tabA.astype(np.float32)
    c["tabB"] = tabB.astype(np.float32)
    c["g64"] = g64.astype(np.float32)
    kd = np.zeros((128, H), np.float64)
    for h in range(H):
        kd[:, h] = np.exp(log_g[h] * (63 - (i % 64))) * 0.125
    c["kdec"] = kd.astype(np.float32)
    rel = np.arange(384) - 255
    bk = _t5_bucket(rel.astype(np.int32))
    oh = np.zeros((32, 384), np.float32)
    oh[bk, np.arange(384)] = 1.0
    c["oh"] = oh
    c["pw"] = np.tile((2.0 ** -np.arange(NIT + 2, dtype=np.float64))[None, :], (128, 1)).astype(np.float32)
    return c


class _Stop(Exception):
    pass


MARKS = []


def build_program(S, NSEQ, DEPTH, NIT=16, dbg=False, stop=None):
    NTS = S // 128
    NT = NSEQ * NTS
    KSEL = min(256, S // 4)
    nc = bass.Bass("TRN2", target_bir_lowering=False)
    es = contextlib.ExitStack()
    P = Prog(nc, es)

    def din(name, shape, dt=F32):
        return nc.dram_tensor(name, list(shape), dt, kind="ExternalInput").ap()

    def dscr(name, shape, dt=F32):
        return nc.dram_tensor(name, list(shape), dt, kind=("ExternalOutput" if dbg else "Internal")).ap()

    x_d = din("x", [NT * 128, D])
    mem_d = din("mem", [NSEQ * 256, D])
    w_in_d = din("w_in", [DEPTH, D, N_IN])
    w_ret_o_d = din("w_ret_o", [DEPTH, 512, D])
    w_dsa_o_d = din("w_dsa_o", [DEPTH, 512, D])
    w_mem_o_d = din("w_mem_o", [DEPTH, 512, D])
    w_out_d = din("w_out", [DEPTH, D, D])
    w_mkv_d = din("w_mem_kv", [DEPTH, D, D])
    w_ff1_d = din("w_ff1", [DEPTH, D, 4096])
    w_ff2_d = din("w_ff2", [DEPTH, 4096, D])
    w_uq_d = din("w_uq", [DEPTH, 256, 512])
    w_iq_d = din("w_iq", [DEPTH, 256, 256])
    w_ukT_d = din("w_ukT", [DEPTH, 1024, 128])
    w_uvp_d = din("w_uvp", [DEPTH, 128, 8 * 128])
    n1T_d = din("norm1T", [128, DEPTH * 8])
    n2T_d = din("norm2T", [128, DEPTH * 8])
    nmT_d = din("mem_normT", [128, DEPTH * 8])
    qnT_d = din("q_normT", [128, DEPTH * 2])
    kvn_d = din("kv_norm", [DEPTH, 128])
    fin_d = din("final_norm", [1, D])
    rb_d = din("rel_bias", [32, 8])
    ident_d = din("ident", [128, 128])
    antiI_d = din("antiI", [128, 128])
    cos_d = din("cos2", [S, 64])
    sin_d = din("sin2", [S, 64])
    dmask_d = din("dmaskT", [128, 1024])
    tabA_d = din("tabA", [128, 512])
    tabB_d = din("tabB", [128, 512])
    g64_d = din("g64", [128, 256])
    kdec_d = din("kdec", [128, 8])
    oh_d = din("oh", [32, 384])
    pw_d = din("pw", [128, NIT + 2])
    out_d = nc.dram_tensor("out", [NT * 128, D], F32, kind="ExternalOutput").ap()
    hT_d = dscr("hT_d", [NT * 128, D], BF16)
    part_d = dscr("part_d", [NT * 128, D])
    x1_d = dscr("x1_d", [NT * 128, D])
    x2_d = dscr("x2_d", [NT * 128, D])
    vrow_d = dscr("vrow_d", [8, 384])

    cnt = [0]

    def sbt(stack, name, shape, dt):
        cnt[0] += 1
        nm = "%s_%d" % (name, cnt[0])
        return TB(stack.enter_context(nc.sbuf_tensor(nm, list(shape), dt)), Buf(nm))

    def MM(out, lhsT, rhs, start, stop, rd, wr):
        P.op("pe", lambda e: e.matmul(out, lhsT=lhsT, rhs=rhs, start=start, stop=stop), rd, wr, mm=True)

    def ACT(out, in_, func, rd, wr, **kw):
        P.op("act", lambda e: e.activation(out=out, in_=in_, func=func, **kw), rd, wr)

    def TT(eng, out, in0, in1, op, rd, wr):
        P.op(eng, lambda e: e.tensor_tensor(out=out, in0=in0, in1=in1, op=op), rd, wr)

    def TS(eng, out, in0, s1, s2, op0, op1, rd, wr, **kw):
        if op1 is None:
            P.op(eng, lambda e: e.tensor_scalar(out=out, in0=in0, scalar1=s1, scalar2=None, op0=op0, **kw), rd, wr)
        else:
            P.op(eng, lambda e: e.tensor_scalar(out=out, in0=in0, scalar1=s1, scalar2=s2, op0=op0, op1=op1, **kw),
                 rd, wr)

    def STT(out, in0, scalar, in1, op0, op1, rd, wr):
        P.op("dve", lambda e: e.scalar_tensor_tensor(out=out, in0=in0, scalar=scalar, in1=in1, op0=op0, op1=op1),
             rd, wr)

    def CP(eng, out, in_, rd, wr):
        if eng == "act":
            P.op("act", lambda e: e.copy(out=out, in_=in_), rd, wr)
        else:
            P.op(eng, lambda e: e.tensor_copy(out=out, in_=in_), rd, wr)

    def RECIP(out, in_, rd, wr):
        P.op("dve", lambda e: e.reciprocal(out=out, in_=in_), rd, wr)

    def MEMSET(eng, ap, val, wr):
        P.op(eng, lambda e: e.memset(ap, val), [], wr)

    def RED(out, in_, op, rd, wr):
        P.op("dve", lambda e: e.tensor_reduce(out=out, in_=in_, op=op, axis=AX.X), rd, wr)

    def MAX8(out, in_, rd, wr):
        P.op("dve", lambda e: e.max(out=out, in_=in_), rd, wr)

    def barrier():
        P.op("pool", lambda e: e.memset(bar.t[:], 0.0), [], [bar.b], barrier=True)

    pb = []
    for i in range(8):
        t = es.enter_context(nc.psum_tensor("pb%d" % i, [128, 512], F32))
        pb.append(TB(t, Buf("pb%d" % i, psum=True)))

    def pbf(i):
        return pb[i].t[:].bitcast(BF16)

    G = es
    bar = sbt(G, "bar", [128, 1], F32)
    identf = sbt(G, "identf", [128, 128], F32)
    identb = sbt(G, "identb", [128, 128], BF16)
    onesb = sbt(G, "onesb", [128, 128], BF16)
    n1T = sbt(G, "n1T", [128, DEPTH * 8], F32)
    n2T = sbt(G, "n2T", [128, DEPTH * 8], F32)
    nmT = sbt(G, "nmT", [128, DEPTH * 8], F32)
    qnT = sbt(G, "qnT", [128, DEPTH * 2], F32)
    biasT = [sbt(G, "biasT%d" % i, [128, 8, 128], BF16) for i in range(2)]
    ss = sbt(G, "ss", [128, 1], F32)
    rstd = sbt(G, "rstd", [128, 1], F32)

    ch_c = P.chan()
    ch_w = P.chan()
    ch_ld = [P.chan() for _ in range(2)]
    ch_ld2 = [P.chan() for _ in range(2)]
    ch_ld3 = [P.chan() for _ in range(2)]
    ch_st = P.chan()
    ch_st2 = P.chan()
    ch_out = P.chan()

    P.dma("sp", identf.t[:], ident_d, [], [identf.b], ch_c)
    P.dma("sp", n1T.t[:], n1T_d, [], [n1T.b], ch_c)
    P.dma("sp", n2T.t[:], n2T_d, [], [n2T.b], ch_c)
    P.dma("sp", nmT.t[:], nmT_d, [], [nmT.b], ch_c)
    P.dma("sp", qnT.t[:], qnT_d, [], [qnT.b], ch_c)
    CP("dve", identb.t[:], identf.t[:], [identf.b], [identb.b])
    MEMSET("pool", onesb.t[:], 1.0, [onesb.b])

    def TR(out, in_, rd, wr):
        P.op("pe", lambda e: e.transpose(out=out, in_=in_, identity=identb.t[:]), rd + [identb.b], wr, mm=True)

    with contextlib.ExitStack() as L:
        rb = sbt(L, "rb", [32, 8], F32)
        oh = sbt(L, "oh", [32, 384], F32)
        anti = sbt(L, "anti", [128, 128], F32)
        vrow = sbt(L, "vrow", [8, 384], F32)
        vfar = sbt(L, "vfar", [8, 1], F32)
        xt = sbt(L, "xt", [128, 8, 128], F32)
        P.dma("sp", rb.t[:], rb_d, [], [rb.b], ch_c)
        P.dma("sp", oh.t[:], oh_d, [], [oh.b], ch_c)
        P.dma("sp", anti.t[:], antiI_d, [], [anti.b], ch_c)
        MM(pb[0].t[0:8, 0:384], rb.t[:], oh.t[:], True, True, [rb.b, oh.b], [pb[0].b])
        CP("dve", vfar.t[:], pb[0].t[0:8, 0:1], [pb[0].b], [vfar.b])
        TS("dve", vrow.t[:], pb[0].t[0:8, 0:384], vfar.t[:, 0:1], None, ALU.subtract, None, [pb[0].b, vfar.b], [vrow.b])
        vrowd_b = Buf("vrowd")
        P.dma("sp", vrow_d, vrow.t[:], [vrow.b], [vrowd_b], ch_st)
        for di in range(2):
            base = 128 if di == 0 else 0
            src = bass.AP(tensor=vrow_d.tensor, offset=base, ap=[[1, 128], [384, 8], [1, 128]])
            P.dma("sp", xt.t[:], src, [vrowd_b], [xt.b], ch_ld[0])
            for h in range(8):
                MM(pb[1 + h // 4].t[:, (h % 4) * 128:(h % 4 + 1) * 128], xt.t[:, h, :], anti.t[:], True, True,
                   [xt.b, anti.b], [pb[1 + h // 4].b])
            for hh in range(2):
                CP("act", biasT[di].t[:, hh * 4:(hh + 1) * 4, :],
                   pb[1 + hh].t[:].rearrange("p (h t) -> p h t", h=4), [pb[1 + hh].b], [biasT[di].b])
        barrier()

    def ckpt(name):
        MARKS.append((name, sum(1 for o in P.ops if o[0] == "pe")))
        if stop == name:
            raise _Stop()

    def _layers():
        for l in range(DEPTH):
            xin_d = x_d if l == 0 else x2_d
            last = (l == DEPTH - 1)

            with contextlib.ExitStack() as L:
                NC1 = 4608
                w1 = sbt(L, "w1", [128, 8, NC1], BF16)
                wro = sbt(L, "wro", [128, 4, D], BF16)
                wmo = sbt(L, "wmo", [128, 4, D], BF16)
                wkv = sbt(L, "wkv", [128, 8, D], BF16)
                colmap = [(C_Q, 2048, 0), (C_MQ, 512, 2048), (C_G0, 1024, 2560), (C_G2, 1024, 3584)]
                for (src0, ncol, dst0) in colmap:
                    for s0 in range(0, ncol, 512):
                        P.dma("pool", w1.t[:, :, dst0 + s0:dst0 + s0 + 512],
                              w_in_d[l, :, src0 + s0:src0 + s0 + 512].rearrange("(c p) n -> p c n", p=128),
                              [], [w1.b], ch_w)
                P.dma("pool", wro.t[:], w_ret_o_d[l].rearrange("(c p) n -> p c n", p=128), [], [wro.b], ch_w)
                P.dma("pool", wmo.t[:], w_mem_o_d[l].rearrange("(c p) n -> p c n", p=128), [], [wmo.b], ch_w)
                for s0 in range(0, D, 512):
                    P.dma("pool", wkv.t[:, :, s0:s0 + 512],
                          w_mkv_d[l, :, s0:s0 + 512].rearrange("(c p) n -> p c n", p=128), [], [wkv.b], ch_w)
                dmask = sbt(L, "dmask", [128, 1024], F32)
                tabA = sbt(L, "tabA", [128, 512], F32)
                tabB = sbt(L, "tabB", [128, 512], F32)
                g64 = sbt(L, "g64", [128, 256], F32)
                kdec = sbt(L, "kdec", [128, 8], F32)
                for tt_, dd_ in ((dmask, dmask_d), (tabA, tabA_d), (tabB, tabB_d), (g64, g64_d), (kdec, kdec_d)):
                    P.dma("sp", tt_.t[:], dd_, [], [tt_.b], ch_c)
                xs = [sbt(L, "xs", [128, D], F32) for _ in range(2)]
                cs = [sbt(L, "cs", [128, 64], F32) for _ in range(2)]
                sn = [sbt(L, "sn", [128, 64], F32) for _ in range(2)]
                junk = sbt(L, "junk", [128, D], BF16)
                hn = sbt(L, "hn", [128, D], BF16)
                hT = sbt(L, "hT", [128, 8, 128], BF16)
                tA = sbt(L, "tA", [128, 512], F32)
                tBm = sbt(L, "tBm", [128, 512], F32)
                q_r = sbt(L, "q_r", [128, 512], BF16)
                k_r = sbt(L, "k_r", [128, 512], BF16)
                kdA = sbt(L, "kdA", [128, 512], BF16)
                kdB = sbt(L, "kdB", [128, 512], BF16)
                v_s = sbt(L, "v_s", [128, 512], BF16)
                sg = sbt(L, "sg", [128, 512], F32)
                qT = sbt(L, "qT", [128, 512], BF16)
                qA = sbt(L, "qA", [128, 512], BF16)
                qB = sbt(L, "qB", [128, 512], BF16)
                kTe = sbt(L, "kTe", [128, 512], BF16)
                kTo = sbt(L, "kTo", [128, 512], BF16)
                sT = sbt(L, "sT", [128, 1024], BF16)
                S32 = [sbt(L, "S32", [128, 256], F32) for _ in range(2)]
                S16 = [sbt(L, "S16", [128, 2, 256], BF16) for _ in range(2)]
                for z_ in (kdA, kdB, kTe, kTo, S16[0], S16[1]):
                    MEMSET("pool", z_.t[:], 0.0, [z_.b])
                stmp = sbt(L, "stmp", [128, 256], F32)
                o_sb = sbt(L, "o_sb", [128, 512], F32)
                o_sq = tA
                st8 = sbt(L, "st8", [128, 32], F32)
                yb = tBm
                ret = sbt(L, "ret", [128, 512], BF16)
                retT = qB
                mqT = qT
                pT = sT
                rs = tBm
                memoT = qA
                g0 = o_sb
                g2 = sg
                t1 = tA
                t2 = tBm
                part = sbt(L, "part", [128, D], F32)
                memT = sbt(L, "memT", [128, 8, 256], BF16)
                KT = sbt(L, "KT", [128, 4, 256], BF16)
                Vm = sbt(L, "Vm", [128, 2, 512], BF16)

                def norm_to_T(xtile, gT, gcol0, dstT, dst_cols, bank):
                    ACT(junk.t[:], xtile.t[:], AF.Square, [xtile.b], [junk.b, ss.b], accum_out=ss.t[:])
                    TS("dve", rstd.t[:], ss.t[:], 1.0 / D, 1e-6, ALU.mult, ALU.add, [ss.b], [rstd.b])
                    ACT(rstd.t[:], rstd.t[:], AF.Sqrt, [rstd.b], [rstd.b])
                    RECIP(rstd.t[:], rstd.t[:], [rstd.b], [rstd.b])
                    TS("dve", hn.t[:], xtile.t[:], rstd.t[:, 0:1], None, ALU.mult, None, [xtile.b, rstd.b], [hn.b])
                    for c in range(8):
                        TR(pbf(bank)[:, c * 128:(c + 1) * 128], hn.t[:, c * 128:(c + 1) * 128], [hn.b], [pb[bank].b])
                    TT("dve", dstT.t[:, :, dst_cols], pbf(bank).rearrange("p (c t) -> p c t", c=8),
                       gT.t[:, gcol0:gcol0 + 8].unsqueeze(2).to_broadcast([128, 8, 128]), ALU.mult,
                       [pb[bank].b, gT.b], [dstT.b])

                def p1_load(n):
                    sl = n % 2
                    t = n % NTS
                    P.dma("sp", xs[sl].t[:], xin_d[n * 128:(n + 1) * 128, :], [], [xs[sl].b], ch_ld[sl])
                    P.dma("sp", cs[sl].t[:], cos_d[t * 128:(t + 1) * 128, :], [], [cs[sl].b], ch_ld2[sl])
                    P.dma("sp", sn[sl].t[:], sin_d[t * 128:(t + 1) * 128, :], [], [sn[sl].b], ch_ld3[sl])

                def rope(bank, dst, sl):
                    v3 = lambda ap: ap.rearrange("p (h d) -> p h d", h=8)
                    cb = cs[sl].t[:].unsqueeze(1).to_broadcast([128, 8, 64])
                    sb_ = sn[sl].t[:].unsqueeze(1).to_broadcast([128, 8, 64])
                    TT("dve", v3(tA.t[:]), v3(pb[bank].t[:]), cb, ALU.mult, [pb[bank].b, cs[sl].b], [tA.b])
                    TT("dve", v3(tBm.t[:]), v3(pb[bank].t[:]), sb_, ALU.mult, [pb[bank].b, sn[sl].b], [tBm.b])
                    TT("pool", v3(dst.t[:])[:, :, 0:32], v3(tA.t[:])[:, :, 0:32], v3(tBm.t[:])[:, :, 32:64], ALU.subtract,
                       [tA.b, tBm.b], [dst.b])
                    TT("pool", v3(dst.t[:])[:, :, 32:64], v3(tA.t[:])[:, :, 32:64], v3(tBm.t[:])[:, :, 0:32], ALU.add,
                       [tA.b, tBm.b], [dst.b])

                def mem_prologue(seq):
                    for mc in range(2):
                        r0 = seq * 256 + mc * 128
                        P.dma("sp", xs[0].t[:], mem_d[r0:r0 + 128, :], [], [xs[0].b], ch_ld[0])
                        norm_to_T(xs[0], nmT, l * 8, memT, slice(mc * 128, (mc + 1) * 128), 0)
                    for h in range(4):
                        bank = 1 + h // 2
                        for c in range(8):
                            MM(pb[bank].t[:, (h % 2) * 256:(h % 2 + 1) * 256], w_kvs(c, h * 128, 128), memT.t[:, c, :],
                               c == 0, c == 7, [wkv.b, memT.b], [pb[bank].b])
                    for hh in range(2):
                        CP("act", KT.t[:, hh * 2:(hh + 1) * 2, :], pb[1 + hh].t[:].rearrange("p (h m) -> p h m", h=2),
                           [pb[1 + hh].b], [KT.b])
                    for mc in range(2):
                        for c in range(8):
                            MM(pb[3 + mc].t[:], memT.t[:, c, mc * 128:(mc + 1) * 128], w_kvs(c, 512, 512),
                               c == 0, c == 7, [wkv.b, memT.b], [pb[3 + mc].b])
                        CP("dve", Vm.t[:, mc, :], pb[3 + mc].t[:], [pb[3 + mc].b], [Vm.b])

                def w_kvs(c, c0, n):
                    return wkv.t[:, c, c0:c0 + n]

                def p1_tile(n):
                    sl = n % 2
                    seq, t = divmod(n, NTS)
                    if t == 0:
                        MEMSET("pool", S32[0].t[:], 0.0, [S32[0].b])
                        MEMSET("pool", S16[0].t[:], 0.0, [S16[0].b])
                    norm_to_T(xs[sl], n1T, l * 8, hT, slice(0, 128), 0)
                    P.dma("sp", hT_d[n * 128:(n + 1) * 128, :], hT.t[:].rearrange("p c t -> p (c t)"), [hT.b], [], ch_st)
                    for blk, bank in ((0, 1), (1, 2), (2, 3), (3, 4)):
                        for c in range(8):
                            MM(pb[bank].t[:], hT.t[:, c, :], w1.t[:, c, blk * 512:(blk + 1) * 512], c == 0, c == 7,
                               [hT.b, w1.b], [pb[bank].b])
                    rope(1, q_r, sl)
                    rope(2, k_r, sl)
                    CP("act", v_s.t[:], pb[3].t[:], [pb[3].b], [v_s.b])
                    ACT(sg.t[:], pb[4].t[:], AF.Silu, [pb[4].b], [sg.b])
                    for kd_, r0 in ((kdA, 0), (kdB, 64)):
                        TT("pool", kd_.t[r0:r0 + 64, :].rearrange("p (h d) -> p h d", h=8),
                           k_r.t[r0:r0 + 64, :].rearrange("p (h d) -> p h d", h=8),
                           kdec.t[r0:r0 + 64, :].unsqueeze(2).to_broadcast([64, 8, 64]), ALU.mult, [k_r.b, kdec.b], [kd_.b])
                    for c in range(4):
                        TR(pbf(0)[:, c * 128:(c + 1) * 128], q_r.t[:, c * 128:(c + 1) * 128], [q_r.b], [pb[0].b])
                    for c in range(4):
                        TR(pbf(0)[:, 512 + c * 128:512 + (c + 1) * 128], k_r.t[:, c * 128:(c + 1) * 128], [k_r.b], [pb[0].b])
                    CP("act", qT.t[:], pbf(0)[:, 0:512], [pb[0].b], [qT.b])
                    TT("dve", qA.t[:], qT.t[:], tabA.t[:], ALU.mult, [qT.b, tabA.b], [qA.b])
                    TT("pool", qB.t[:], qT.t[:], tabB.t[:], ALU.mult, [qT.b, tabB.b], [qB.b])
                    CP("act", kTe.t[0:64, :], pbf(0)[0:64, 512:1024], [pb[0].b], [kTe.b])
                    CP("act", kTo.t[64:128, :], pbf(0)[64:128, 512:1024], [pb[0].b], [kTo.b])
                    for h in range(8):
                        p_, b_ = h // 2, 64 * (h % 2)
                        bank = 1 + h // 4
                        kT_ = kTe if h % 2 == 0 else kTo
                        MM(pb[bank].t[:, (h % 4) * 128:(h % 4 + 1) * 128], kT_.t[:, p_ * 128:(p_ + 1) * 128],
                           qT.t[:, p_ * 128:(p_ + 1) * 128], True, True, [kT_.b, qT.b], [pb[bank].b])
                    for hh in range(2):
                        TT("dve", sT.t[:, hh * 512:(hh + 1) * 512], pb[1 + hh].t[:], dmask.t[:, hh * 512:(hh + 1) * 512],
                           ALU.mult, [pb[1 + hh].b, dmask.b], [sT.b])
                    for ci in range(2):
                        r0 = 64 * ci
                        bank = 3 + ci
                        kd_ = kdA if ci == 0 else kdB
                        for p_ in range(4):
                            MM(pb[bank].t[:, p_ * 128:(p_ + 1) * 128], kd_.t[:, p_ * 128:(p_ + 1) * 128],
                               v_s.t[:, p_ * 128:(p_ + 1) * 128], True, True, [kd_.b, v_s.b], [pb[bank].b])
                        src, dst = S32[ci], S32[1 - ci]
                        TT("dve", stmp.t[:], src.t[:], g64.t[:], ALU.mult, [src.b, g64.b], [stmp.b])
                        kv4 = pb[bank].t[:].rearrange("p (a e) -> p a e", a=4)
                        d3 = dst.t[:].rearrange("p (a e) -> p a e", a=4)
                        s3 = stmp.t[:].rearrange("p (a e) -> p a e", a=4)
                        TT("dve", d3[0:64], s3[0:64], kv4[0:64, :, 0:64], ALU.add, [stmp.b, pb[bank].b], [dst.b])
                        TT("dve", d3[64:128], s3[64:128], kv4[64:128, :, 64:128], ALU.add, [stmp.b, pb[bank].b], [dst.b])
                        if ci == 0:
                            CP("pool", S16[1].t[0:64, 0, :], S32[1].t[0:64, :], [S32[1].b], [S16[1].b])
                            CP("pool", S16[1].t[64:128, 1, :], S32[1].t[64:128, :], [S32[1].b], [S16[1].b])
                    for h in range(8):
                        p_, b_ = h // 2, 64 * (h % 2)
                        o_ap = pb[5].t[:, h * 64:(h + 1) * 64]
                        MM(o_ap, sT.t[:, h * 128:(h + 1) * 128], v_s.t[:, h * 64:(h + 1) * 64], True, False,
                           [sT.b, v_s.b], [pb[5].b])
                        MM(o_ap, qA.t[:, p_ * 128:(p_ + 1) * 128], S16[0].t[:, h % 2, p_ * 64:(p_ + 1) * 64],
                           False, False, [qA.b, S16[0].b], [pb[5].b])
                        MM(o_ap, qB.t[:, p_ * 128:(p_ + 1) * 128], S16[1].t[:, h % 2, p_ * 64:(p_ + 1) * 64],
                           False, True, [qB.b, S16[1].b], [pb[5].b])
                    CP("pool", S16[0].t[0:64, 0, :], S32[0].t[0:64, :], [S32[0].b], [S16[0].b])
                    CP("pool", S16[0].t[64:128, 1, :], S32[0].t[64:128, :], [S32[0].b], [S16[0].b])
                    CP("act", o_sb.t[:], pb[5].t[:], [pb[5].b], [o_sb.b])
                    ACT(o_sq.t[:], pb[5].t[:], AF.Square, [pb[5].b], [o_sq.b])
                    o3 = o_sb.t[:].rearrange("p (h e) -> p h e", h=8)
                    RED(st8.t[:, 0:8], o3, ALU.add, [o_sb.b], [st8.b])
                    RED(st8.t[:, 8:16], o_sq.t[:].rearrange("p (h e) -> p h e", h=8), ALU.add, [o_sq.b], [st8.b])
                    TS("dve", st8.t[:, 16:24], st8.t[:, 0:8], 1.0 / 64, None, ALU.mult, None, [st8.b], [st8.b])
                    TT("dve", st8.t[:, 0:8], st8.t[:, 16:24], st8.t[:, 16:24], ALU.mult, [st8.b], [st8.b])
                    STT(st8.t[:, 24:32], st8.t[:, 8:16], 1.0 / 64, st8.t[:, 0:8], ALU.mult, ALU.subtract, [st8.b], [st8.b])
                    TS("dve", st8.t[:, 24:32], st8.t[:, 24:32], 1e-5, None, ALU.add, None, [st8.b], [st8.b])
                    ACT(st8.t[:, 24:32], st8.t[:, 24:32], AF.Sqrt, [st8.b], [st8.b])
                    RECIP(st8.t[:, 24:32], st8.t[:, 24:32], [st8.b], [st8.b])
                    y3 = yb.t[:].rearrange("p (h e) -> p h e", h=8)
                    TT("dve", y3, o3, st8.t[:, 16:24].unsqueeze(2).to_broadcast([128, 8, 64]), ALU.subtract,
                       [o_sb.b, st8.b], [yb.b])
                    TT("pool", y3, y3, st8.t[:, 24:32].unsqueeze(2).to_broadcast([128, 8, 64]), ALU.mult,
                       [yb.b, st8.b], [yb.b])
                    TT("pool", ret.t[:], yb.t[:], sg.t[:], ALU.mult, [yb.b, sg.b], [ret.b])
                    for c in range(4):
                        TR(pbf(0)[:, c * 128:(c + 1) * 128], ret.t[:, c * 128:(c + 1) * 128], [ret.b], [pb[0].b])
                    CP("act", retT.t[:], pbf(0)[:, 0:512], [pb[0].b], [retT.b])
                    for j in range(4):
                        for c in range(8):
                            MM(pb[1].t[:, j * 128:(j + 1) * 128], w1.t[:, c, 2048 + j * 128:2048 + (j + 1) * 128],
                               hT.t[:, c, :], c == 0, c == 7, [w1.b, hT.b], [pb[1].b])
                    CP("dve", mqT.t[:], pb[1].t[:], [pb[1].b], [mqT.b])
                    for mc in range(2):
                        for h in range(4):
                            MM(pb[2 + mc].t[:, h * 128:(h + 1) * 128], KT.t[:, h, mc * 128:(mc + 1) * 128],
                               mqT.t[:, h * 128:(h + 1) * 128], True, True, [KT.b, mqT.b], [pb[2 + mc].b])
                        ACT(pT.t[:, mc * 512:(mc + 1) * 512], pb[2 + mc].t[:], AF.Exp, [pb[2 + mc].b], [pT.b],
                            scale=128.0 ** -0.5)
                    for h in range(4):
                        for mc in range(2):
                            MM(pb[4].t[:, h * 128:(h + 1) * 128], Vm.t[:, mc, h * 128:(h + 1) * 128],
                               pT.t[:, mc * 512 + h * 128:mc * 512 + (h + 1) * 128], mc == 0, mc == 1,
                               [Vm.b, pT.b], [pb[4].b])
                    for mc in range(2):
                        MM(pb[5].t[:], onesb.t[:], pT.t[:, mc * 512:(mc + 1) * 512], mc == 0, mc == 1,
                           [onesb.b, pT.b], [pb[5].b])
                    RECIP(rs.t[:], pb[5].t[:], [pb[5].b], [rs.b])
                    TT("dve", memoT.t[:], pb[4].t[:], rs.t[:], ALU.mult, [pb[4].b, rs.b], [memoT.b])
                    for blk in range(2):
                        cs_ = slice(blk * 512, (blk + 1) * 512)
                        for c in range(4):
                            MM(pb[6].t[:], retT.t[:, c * 128:(c + 1) * 128], wro.t[:, c, cs_], c == 0, c == 3,
                               [retT.b, wro.b], [pb[6].b])
                        for c in range(4):
                            MM(pb[7].t[:], memoT.t[:, c * 128:(c + 1) * 128], wmo.t[:, c, cs_], c == 0, c == 3,
                               [memoT.b, wmo.b], [pb[7].b])
                        for c in range(8):
                            MM(pb[2].t[:], hT.t[:, c, :], w1.t[:, c, 2560 + blk * 512:2560 + (blk + 1) * 512], c == 0, c == 7,
                               [hT.b, w1.b], [pb[2].b])
                        for c in range(8):
                            MM(pb[3].t[:], hT.t[:, c, :], w1.t[:, c, 3584 + blk * 512:3584 + (blk + 1) * 512], c == 0, c == 7,
                               [hT.b, w1.b], [pb[3].b])
                        ACT(g0.t[:], pb[2].t[:], AF.Sigmoid, [pb[2].b], [g0.b])
                        ACT(g2.t[:], pb[3].t[:], AF.Sigmoid, [pb[3].b], [g2.b])
                        TT("dve", t1.t[:], pb[6].t[:], g0.t[:], ALU.mult, [pb[6].b, g0.b], [t1.b])
                        TT("dve", t2.t[:], pb[7].t[:], g2.t[:], ALU.mult, [pb[7].b, g2.b], [t2.b])
                        TT("pool", part.t[:, cs_], t1.t[:], t2.t[:], ALU.add, [t1.b, t2.b], [part.b])
                    P.dma("sp", part_d[n * 128:(n + 1) * 128, :], part.t[:], [part.b], [], ch_st2)

                ckpt("p1w")
                for n in range(NT):
                    if n % NTS == 0:
                        mem_prologue(n // NTS)
                        ckpt("p1m")
                        p1_load(n)
                    if (n + 1) % NTS != 0:
                        p1_load(n + 1)
                    p1_tile(n)
                    ckpt("p1t")
                barrier()
                ckpt("p1_%d" % l)

            with contextlib.ExitStack() as L:
                NC2 = 424 + 1024
                w2 = sbt(L, "w2", [128, 8, NC2], BF16)
                wuq = sbt(L, "wuq", [128, 2, 512], BF16)
                wiq = sbt(L, "wiq", [128, 2, 256], BF16)
                wuk = sbt(L, "wuk", [128, 8, 128], BF16)
                wuv = sbt(L, "wuv", [128, 8 * 128], BF16)
                wdo = sbt(L, "wdo", [128, 4, D], BF16)
                wo = sbt(L, "wo", [128, 8, D], BF16)
                P.dma("pool", w2.t[:, :, 0:424], w_in_d[l, :, C_CQ:C_CQ + 424].rearrange("(c p) n -> p c n", p=128),
                      [], [w2.b], ch_w)
                for s0 in range(0, 1024, 512):
                    P.dma("pool", w2.t[:, :, 424 + s0:424 + s0 + 512],
                          w_in_d[l, :, C_G1 + s0:C_G1 + s0 + 512].rearrange("(c p) n -> p c n", p=128), [], [w2.b], ch_w)
                P.dma("pool", wuq.t[:], w_uq_d[l].rearrange("(c p) n -> p c n", p=128), [], [wuq.b], ch_w)
                P.dma("pool", wiq.t[:], w_iq_d[l].rearrange("(c p) n -> p c n", p=128), [], [wiq.b], ch_w)
                P.dma("pool", wuk.t[:], w_ukT_d[l].rearrange("(c p) n -> p c n", p=128), [], [wuk.b], ch_w)
                P.dma("pool", wuv.t[:], w_uvp_d[l], [], [wuv.b], ch_w)
                P.dma("pool", wdo.t[:], w_dsa_o_d[l].rearrange("(c p) n -> p c n", p=128), [], [wdo.b], ch_w)
                for s0 in range(0, D, 512):
                    P.dma("pool", wo.t[:, :, s0:s0 + 512],
                          w_out_d[l, :, s0:s0 + 512].rearrange("(c p) n -> p c n", p=128), [], [wo.b], ch_w)
                gkv = sbt(L, "gkv", [128, 128], F32)
                P.dma("sp", gkv.t[:], kvn_d[l:l + 1, :].to_broadcast([128, 128]), [], [gkv.b], ch_c)
                pw = sbt(L, "pw", [128, NIT + 2], F32)
                P.dma("sp", pw.t[:], pw_d, [], [pw.b], ch_c)
                hTs = [sbt(L, "hTs", [128, 8, 128], BF16) for _ in range(2)]
                pts = [sbt(L, "pts", [128, D], F32) for _ in range(2)]
                xs = [sbt(L, "xs2", [128, D], F32) for _ in range(2)]
                ckv = sbt(L, "ckv", [128, NTS, 128], BF16)
                ckvT = sbt(L, "ckvT", [128, S], BF16)
                kiT = sbt(L, "kiT", [32, S], BF16)
                acc = sbt(L, "acc", [128, S], F32)
                nm = sbt(L, "nm", [128, S], BF16)
                nmT_ = sbt(L, "nmT_", [128, NTS, 128], BF16)
                rt = [sbt(L, "rt", [128, 512], F32) for _ in range(2)]
                cqT = sbt(L, "cqT", [128, 2, 128], BF16)
                sq = sbt(L, "sq", [128, 2, 128], BF16)
                rq8 = sbt(L, "rq8", [128, 128], F32)
                qTd = sbt(L, "qTd", [128, 4, 128], BF16)
                qlat2 = [sbt(L, "qlat", [128, 8, 128], BF16) for _ in range(2)]
                qiT = sbt(L, "qiT", [32, 8, 128], BF16)
                iw = sbt(L, "iw", [128, 8], F32)
                jk = sbt(L, "jk", [128, 128], F32)
                g1s = [sbt(L, "g1", [128, D], F32) for _ in range(2)]
                th = sbt(L, "th", [128, 8], F32)
                mx8 = sbt(L, "mx8", [128, 8], F32)
                steps = sbt(L, "steps", [128, NIT + 2], F32)
                pTs = [sbt(L, "pTs", [128, 512], BF16) for _ in range(2)]
                rs2 = sbt(L, "rs2", [128, 512], F32)
                olat = sbt(L, "olat", [128, 8, 128], BF16)
                dsaT = sbt(L, "dsaT", [128, 4, 128], BF16)
                m1 = sbt(L, "m1", [128, D], F32)
                mg = sbt(L, "mg", [128, D], BF16)
                mT = sbt(L, "mT", [128, 8, 128], BF16)
                x1 = sbt(L, "x1", [128, D], F32)

                def p2_load_h(n):
                    sl = n % 2
                    P.dma("sp", hTs[sl].t[:].rearrange("p c t -> p (c t)"), hT_d[n * 128:(n + 1) * 128, :], [],
                          [hTs[sl].b], ch_ld[sl])

                def p2_load_px(n):
                    sl = n % 2
                    P.dma("sp", pts[sl].t[:], part_d[n * 128:(n + 1) * 128, :], [], [pts[sl].b], ch_ld2[sl])
                    P.dma("sp", xs[sl].t[:], xin_d[n * 128:(n + 1) * 128, :], [], [xs[sl].b], ch_ld3[sl])

                def p2_part(n, part):
                    sl = n % 2
                    seq, t = divmod(n, NTS)
                    hT = hTs[sl]
                    NK = 128 * (t + 1)
                    qlat = qlat2[sl]
                    g1 = g1s[sl]
                    if part == "A":
                        for j in range(2):
                            for c in range(8):
                                MM(pb[7].t[:, j * 128:(j + 1) * 128], w2.t[:, c, j * 128:(j + 1) * 128], hT.t[:, c, :],
                                   c == 0, c == 7, [w2.b, hT.b], [pb[7].b])
                        for j in range(2):
                            TS("dve", cqT.t[:, j, :], pb[7].t[:, j * 128:(j + 1) * 128], qnT.t[:, l * 2 + j:l * 2 + j + 1], None,
                               ALU.mult, None, [pb[7].b, qnT.b], [cqT.b])
                        ACT(sq.t[:].rearrange("p a t -> p (a t)"), pb[7].t[:, 0:256], AF.Square, [pb[7].b], [sq.b])
                        for j in range(2):
                            MM(pb[6].t[:, 0:128], onesb.t[:], sq.t[:, j, :], j == 0, j == 1, [onesb.b, sq.b], [pb[6].b])
                        TS("dve", rq8.t[:], pb[6].t[:, 0:128], 0.25, 64e-6, ALU.mult, ALU.add, [pb[6].b], [rq8.b])
                        ACT(rq8.t[:], rq8.t[:], AF.Sqrt, [rq8.b], [rq8.b])
                        RECIP(rq8.t[:], rq8.t[:], [rq8.b], [rq8.b])
                        for j in range(4):
                            for rc in range(2):
                                MM(pb[5].t[:, j * 128:(j + 1) * 128], wuq.t[:, rc, j * 128:(j + 1) * 128], cqT.t[:, rc, :],
                                   rc == 0, rc == 1, [wuq.b, cqT.b], [pb[5].b])
                        CP("act", qTd.t[:].rearrange("p a t -> p (a t)"), pb[5].t[:], [pb[5].b], [qTd.b])
                        for h in range(8):
                            p_, b_ = h // 2, 64 * (h % 2)
                            bank = 2 + h // 4
                            MM(pb[bank].t[:, (h % 4) * 128:(h % 4 + 1) * 128], wuk.t[:, (h % 2) * 4 + p_, :],
                               qTd.t[:, p_, :], True, True, [wuk.b, qTd.b], [pb[bank].b])
                        for hh in range(2):
                            TT("dve", qlat.t[:, hh * 4:(hh + 1) * 4, :], pb[2 + hh].t[:].rearrange("p (h t) -> p h t", h=4),
                               rq8.t[:].unsqueeze(1).to_broadcast([128, 4, 128]), ALU.mult, [pb[2 + hh].b, rq8.b], [qlat.b])
                        for h in range(8):
                            bank = (4, 6)[h // 4]
                            for rc in range(2):
                                MM(pb[bank].t[0:32, (h % 4) * 128:(h % 4 + 1) * 128], wiq.t[:, rc, h * 32:(h + 1) * 32],
                                   cqT.t[:, rc, :], rc == 0, rc == 1, [wiq.b, cqT.b], [pb[bank].b])
                        for hh in range(2):
                            CP("act", qiT.t[:, hh * 4:(hh + 1) * 4, :], pb[(4, 6)[hh]].t[0:32, :].rearrange("p (h t) -> p h t", h=4),
                               [pb[(4, 6)[hh]].b], [qiT.b])
                        for c in range(8):
                            MM(pb[1].t[:, 0:128], hT.t[:, c, :], w2.t[:, c, 256:384], c == 0, c == 7, [hT.b, w2.b], [pb[1].b])
                        ACT(jk.t[:], pb[1].t[:, 0:128], AF.Square, [pb[1].b], [jk.b, ss.b], accum_out=ss.t[:])
                        TS("dve", rstd.t[:], ss.t[:], 1.0 / 128, 1e-6, ALU.mult, ALU.add, [ss.b], [rstd.b])
                        ACT(rstd.t[:], rstd.t[:], AF.Sqrt, [rstd.b], [rstd.b])
                        RECIP(rstd.t[:], rstd.t[:], [rstd.b], [rstd.b])
                        STT(ckv.t[:, t, :], pb[1].t[:, 0:128], rstd.t[:, 0:1], gkv.t[:], ALU.mult, ALU.mult,
                            [pb[1].b, rstd.b, gkv.b], [ckv.b])
                        TR(pbf(0)[:, 0:128], ckv.t[:, t, :], [ckv.b], [pb[0].b])
                        CP("act", ckvT.t[:, t * 128:(t + 1) * 128], pbf(0)[:, 0:128], [pb[0].b], [ckvT.b])
                        for c in range(8):
                            MM(pb[1].t[0:32, 128:256], w2.t[:, c, 384:416], hT.t[:, c, :], c == 0, c == 7, [w2.b, hT.b], [pb[1].b])
                        for c in range(8):
                            MM(pb[1].t[:, 256:264], hT.t[:, c, :], w2.t[:, c, 416:424], c == 0, c == 7, [hT.b, w2.b], [pb[1].b])
                        CP("act", kiT.t[:, t * 128:(t + 1) * 128], pb[1].t[0:32, 128:256], [pb[1].b], [kiT.b])
                        CP("dve", iw.t[:], pb[1].t[:, 256:264], [pb[1].b], [iw.b])
                        for blk in range(2):
                            for c in range(8):
                                MM(pb[6 + blk].t[:], hT.t[:, c, :], w2.t[:, c, 424 + blk * 512:424 + (blk + 1) * 512], c == 0, c == 7,
                                   [hT.b, w2.b], [pb[6 + blk].b])
                            ACT(g1.t[:, blk * 512:(blk + 1) * 512], pb[6 + blk].t[:], AF.Sigmoid, [pb[6 + blk].b], [g1.b])
                        kb = 0
                        ctr = 0
                        while kb < NK:
                            wk = min(512, NK - kb)
                            for h in range(8):
                                bank = ctr % 2
                                MM(pb[bank].t[:, 0:wk], qiT.t[:, h, :], kiT.t[:, kb:kb + wk], True, True, [qiT.b, kiT.b],
                                   [pb[bank].b])
                                ACT(rt[bank].t[:, 0:wk], pb[bank].t[:, 0:wk], AF.Relu, [pb[bank].b], [rt[bank].b])
                                if h == 0:
                                    TS("dve", acc.t[:, kb:kb + wk], rt[bank].t[:, 0:wk], iw.t[:, 0:1], None, ALU.mult, None,
                                       [rt[bank].b, iw.b], [acc.b])
                                else:
                                    STT(acc.t[:, kb:kb + wk], rt[bank].t[:, 0:wk], iw.t[:, h:h + 1], acc.t[:, kb:kb + wk],
                                        ALU.mult, ALU.add, [rt[bank].b, iw.b, acc.b], [acc.b])
                                ctr += 1
                            kb += wk
                    if part == "Bd":
                        if NK > KSEL:
                            MAX8(mx8.t[:], acc.t[:, 0:NK - 64], [acc.b], [mx8.b])
                            RED(th.t[:, 1:2], acc.t[:, 0:NK - 64], ALU.min, [acc.b], [th.b])
                            MAX8(mx8.t[64:128, :], acc.t[64:128, 0:NK], [acc.b], [mx8.b])
                            MEMSET("dve", acc.t[0:64, NK - 64:NK], -1e30, [acc.b])
                            TT("dve", th.t[:, 0:1], mx8.t[:, 0:1], th.t[:, 1:2], ALU.add, [mx8.b, th.b], [th.b])
                            TS("dve", th.t[:, 0:1], th.t[:, 0:1], 0.5, None, ALU.mult, None, [th.b], [th.b])
                            TT("dve", th.t[:, 2:3], mx8.t[:, 0:1], th.t[:, 1:2], ALU.subtract, [mx8.b, th.b], [th.b])
                            TS("dve", th.t[:, 2:3], th.t[:, 2:3], 0.5000001, 1e-30, ALU.mult, ALU.add, [th.b], [th.b])
                            TS("dve", steps.t[:], pw.t[:], th.t[:, 2:3], None, ALU.mult, None, [pw.b, th.b], [steps.b])
                            for k in range(NIT):
                                TS("dve", nm.t[:, 0:NK], acc.t[:, 0:NK], th.t[:, 0:1], 0.0, ALU.is_ge, ALU.add,
                                   [acc.b, th.b], [nm.b, th.b], accum_out=th.t[:, 3:4])
                                TS("dve", th.t[:, 4:5], th.t[:, 3:4], float(KSEL), 0.5, ALU.is_ge, ALU.subtract, [th.b], [th.b])
                                STT(th.t[:, 0:1], th.t[:, 4:5], steps.t[:, k:k + 1], th.t[:, 0:1], ALU.mult, ALU.add,
                                    [th.b, steps.b], [th.b])
                            TT("dve", th.t[:, 5:6], th.t[:, 0:1], steps.t[:, NIT:NIT + 1], ALU.subtract, [th.b, steps.b], [th.b])
                        else:
                            MEMSET("dve", acc.t[0:64, NK - 64:NK], -1e30, [acc.b])
                            MEMSET("dve", th.t[:, 5:6], -1e29, [th.b])
                        TS("dve", nm.t[:, 0:NK], acc.t[:, 0:NK], th.t[:, 5:6], NEG, ALU.is_lt, ALU.mult, [acc.b, th.b], [nm.b])
                    if part == "Bp":
                        for kt0 in range(0, t + 1, 8):
                            nblk = min(8, t + 1 - kt0)
                            for i in range(nblk):
                                kt = kt0 + i
                                TR(pbf(7)[:, i * 128:(i + 1) * 128], nm.t[:, kt * 128:(kt + 1) * 128], [nm.b], [pb[7].b])
                            CP("act", nmT_.t[:, kt0:kt0 + nblk, :].rearrange("p a t -> p (a t)"), pbf(7)[:, 0:nblk * 128],
                               [pb[7].b], [nmT_.b])
                    if part == "Ca":
                        ctr = 0
                        for kt in range(t + 1):
                            for half in range(2):
                                bank = half
                                sl2 = ctr % 2
                                MM(pb[bank].t[:], ckvT.t[:, kt * 128:(kt + 1) * 128],
                                   qlat.t[:, half * 4:(half + 1) * 4, :].rearrange("p h t -> p (h t)"), True, False,
                                   [ckvT.b, qlat.b], [pb[bank].b])
                                near = kt >= t - 1
                                MM(pb[bank].t[:].rearrange("p (h t) -> p h t", h=4), identb.t[:],
                                   nmT_.t[:, kt, :].unsqueeze(1).to_broadcast([128, 4, 128]), False, not near,
                                   [identb.b, nmT_.b], [pb[bank].b])
                                if near:
                                    MM(pb[bank].t[:].rearrange("p (h t) -> p h t", h=4), identb.t[:],
                                       biasT[t - kt].t[:, half * 4:(half + 1) * 4, :], False, True, [identb.b, biasT[t - kt].b],
                                       [pb[bank].b])
                                ACT(pTs[sl2].t[:], pb[bank].t[:], AF.Exp, [pb[bank].b], [pTs[sl2].b])
                                MM(pb[2 + half].t[:], ckv.t[:, kt, :], pTs[sl2].t[:], kt == 0, kt == t, [ckv.b, pTs[sl2].b],
                                   [pb[2 + half].b])
                                MM(pb[4 + half].t[:], onesb.t[:], pTs[sl2].t[:], kt == 0, kt == t, [onesb.b, pTs[sl2].b],
                                   [pb[4 + half].b])
                                ctr += 1
                    if part == "Ct":
                        for half in range(2):
                            RECIP(rs2.t[:], pb[4 + half].t[:], [pb[4 + half].b], [rs2.b])
                            TT("dve", olat.t[:, half * 4:(half + 1) * 4, :].rearrange("p h t -> p (h t)"), pb[2 + half].t[:],
                               rs2.t[:], ALU.mult, [pb[2 + half].b, rs2.b], [olat.b])
                        for p_ in range(4):
                            for hi in range(2):
                                h = 2 * p_ + hi
                                MM(pb[6].t[:, p_ * 128:(p_ + 1) * 128], wuv.t[:, h * 128:(h + 1) * 128], olat.t[:, h, :],
                                   hi == 0, hi == 1, [wuv.b, olat.b], [pb[6].b])
                        CP("act", dsaT.t[:].rearrange("p a t -> p (a t)"), pb[6].t[:], [pb[6].b], [dsaT.b])
                        for blk in range(2):
                            cs_ = slice(blk * 512, (blk + 1) * 512)
                            for c in range(4):
                                MM(pb[6].t[:], dsaT.t[:, c, :], wdo.t[:, c, cs_], c == 0, c == 3, [dsaT.b, wdo.b], [pb[6].b])
                            TT("dve", m1.t[:, cs_], pb[6].t[:], g1.t[:, cs_], ALU.mult, [pb[6].b, g1.b], [m1.b])
                        TT("pool", mg.t[:], m1.t[:], pts[sl].t[:], ALU.add, [m1.b, pts[sl].b], [mg.b])
                        for c in range(8):
                            TR(pbf(7)[:, c * 128:(c + 1) * 128], mg.t[:, c * 128:(c + 1) * 128], [mg.b], [pb[7].b])
                        CP("act", mT.t[:].rearrange("p c t -> p (c t)"), pbf(7)[:, :], [pb[7].b], [mT.b])
                        for blk in range(2):
                            cs_ = slice(blk * 512, (blk + 1) * 512)
                            for c in range(8):
                                MM(pb[6].t[:], mT.t[:, c, :], wo.t[:, c, cs_], c == 0, c == 7, [mT.b, wo.b], [pb[6].b])
                            TT("dve", x1.t[:, cs_], pb[6].t[:], xs[sl].t[:, cs_], ALU.add, [pb[6].b, xs[sl].b], [x1.b])
                        P.dma("sp", x1_d[n * 128:(n + 1) * 128, :], x1.t[:], [x1.b], [], ch_st)

                ckpt("p2w")
                for seq_ in range(NSEQ):
                    n0_ = seq_ * NTS
                    p2_load_h(n0_)
                    for t_ in range(NTS):
                        n = n0_ + t_
                        if t_ + 1 < NTS:
                            p2_load_h(n + 1)
                        if t_ > 0:
                            p2_load_px(n - 1)
                        p2_part(n, "A")
                        p2_part(n, "Bd")
                        if t_ > 0:
                            p2_part(n - 1, "Ca")
                        p2_part(n, "Bp")
                        if t_ > 0:
                            p2_part(n - 1, "Ct")
                        ckpt("p2t%d" % n)
                    last_ = n0_ + NTS - 1
                    p2_load_px(last_)
                    p2_part(last_, "Ca")
                    p2_part(last_, "Ct")
                barrier()
                ckpt("p2_%d" % l)

            with contextlib.ExitStack() as L:
                wf1 = sbt(L, "wf1", [128, 8, 4096], BF16)
                wf2 = sbt(L, "wf2", [128, 32, D], BF16)
                for s0 in range(0, 4096, 512):
                    P.dma("pool", wf1.t[:, :, s0:s0 + 512],
                          w_ff1_d[l, :, s0:s0 + 512].rearrange("(c p) n -> p c n", p=128), [], [wf1.b], ch_w)
                for c0 in range(0, 32, 4):
                    P.dma("pool", wf2.t[:, c0:c0 + 4, :],
                          w_ff2_d[l, c0 * 128:(c0 + 4) * 128, :].rearrange("(c p) n -> p c n", p=128), [], [wf2.b], ch_w)
                xs = [sbt(L, "xs4", [128, D], F32) for _ in range(2)]
                junk = sbt(L, "junk4", [128, D], BF16)
                hn = sbt(L, "hn4", [128, D], BF16)
                h2T = sbt(L, "h2T", [128, 8, 128], BF16)
                rl = [sbt(L, "rl", [128, 512], F32) for _ in range(2)]
                uT = sbt(L, "uT", [128, 32, 128], BF16)
                x2 = sbt(L, "x2", [128, D], F32)
                fo = sbt(L, "fo", [128, D], F32)
                if last:
                    gfin = sbt(L, "gfin", [128, D], F32)
                    P.dma("sp", gfin.t[:], fin_d.to_broadcast([128, D]), [], [gfin.b], ch_c)

                def p4_load(n):
                    sl = n % 2
                    P.dma("sp", xs[sl].t[:], x1_d[n * 128:(n + 1) * 128, :], [], [xs[sl].b], ch_ld[sl])

                def p4_tile(n):
                    sl = n % 2
                    xt_ = xs[sl]
                    ACT(junk.t[:], xt_.t[:], AF.Square, [xt_.b], [junk.b, ss.b], accum_out=ss.t[:])
                    TS("dve", rstd.t[:], ss.t[:], 1.0 / D, 1e-6, ALU.mult, ALU.add, [ss.b], [rstd.b])
                    ACT(rstd.t[:], rstd.t[:], AF.Sqrt, [rstd.b], [rstd.b])
                    RECIP(rstd.t[:], rstd.t[:], [rstd.b], [rstd.b])
                    TS("dve", hn.t[:], xt_.t[:], rstd.t[:, 0:1], None, ALU.mult, None, [xt_.b, rstd.b], [hn.b])
                    for c in range(8):
                        TR(pbf(0)[:, c * 128:(c + 1) * 128], hn.t[:, c * 128:(c + 1) * 128], [hn.b], [pb[0].b])
                    TT("dve", h2T.t[:], pbf(0).rearrange("p (c t) -> p c t", c=8),
                       n2T.t[:, l * 8:l * 8 + 8].unsqueeze(2).to_broadcast([128, 8, 128]), ALU.mult, [pb[0].b, n2T.b],
                       [h2T.b])
                    for fg in range(8):
                        bank = 1 + fg % 4
                        for j in range(4):
                            fc = fg * 4 + j
                            for c in range(8):
                                MM(pb[bank].t[:, j * 128:(j + 1) * 128], wf1.t[:, c, fc * 128:(fc + 1) * 128], h2T.t[:, c, :],
                                   c == 0, c == 7, [wf1.b, h2T.b], [pb[bank].b])
                        r_ = rl[fg % 2]
                        ACT(r_.t[:], pb[bank].t[:], AF.Relu, [pb[bank].b], [r_.b])
                        TT("dve", uT.t[:, fg * 4:(fg + 1) * 4, :].rearrange("p a t -> p (a t)"),
                           r_.t[:], r_.t[:], ALU.mult, [r_.b], [uT.b])
                    for blk in range(2):
                        cs_ = slice(blk * 512, (blk + 1) * 512)
                        bank = 5 + blk
                        for fc in range(32):
                            MM(pb[bank].t[:], uT.t[:, fc, :], wf2.t[:, fc, cs_], fc == 0, fc == 31, [uT.b, wf2.b],
                               [pb[bank].b])
                        TT("dve", x2.t[:, cs_], pb[bank].t[:], xt_.t[:, cs_], ALU.add, [pb[bank].b, xt_.b], [x2.b])
                    if not last:
                        P.dma("sp", x2_d[n * 128:(n + 1) * 128, :], x2.t[:], [x2.b], [], ch_st)
                    else:
                        if dbg:
                            P.dma("sp", x2_d[n * 128:(n + 1) * 128, :], x2.t[:], [x2.b], [], ch_st)
                        ACT(junk.t[:], x2.t[:], AF.Square, [x2.b], [junk.b, ss.b], accum_out=ss.t[:])
                        TS("dve", rstd.t[:], ss.t[:], 1.0 / D, 1e-6, ALU.mult, ALU.add, [ss.b], [rstd.b])
                        ACT(rstd.t[:], rstd.t[:], AF.Sqrt, [rstd.b], [rstd.b])
                        RECIP(rstd.t[:], rstd.t[:], [rstd.b], [rstd.b])
                        STT(fo.t[:], x2.t[:], rstd.t[:, 0:1], gfin.t[:], ALU.mult, ALU.mult, [x2.b, rstd.b, gfin.b], [fo.b])
                        P.dma("sp", out_d[n * 128:(n + 1) * 128, :], fo.t[:], [fo.b], [], ch_out)

                p4_load(0)
                for n in range(NT):
                    if n + 1 < NT:
                        p4_load(n + 1)
                    p4_tile(n)
                barrier()
                ckpt("p4_%d" % l)


    try:
        ckpt("bias")
        _layers()
    except _Stop:
        pass
    stats = P.emit(final_chans=[ch_out, ch_st, ch_st2])
    return nc, es, stats


def prep_shared(inputs, S, DEPTH, NIT):
    f = lambda a: np.ascontiguousarray(np.asarray(a, dtype=np.float32))
    sh = {}
    for k in ("w_in", "w_ret_o", "w_dsa_o", "w_mem_o", "w_out", "w_mem_kv", "w_ff1", "w_ff2", "rel_bias", "kv_norm"):
        sh[k] = f(inputs[k])
    sh["w_uq"] = f(np.asarray(inputs["w_uq"]).reshape(DEPTH, 256, 512))
    sh["w_iq"] = f(np.asarray(inputs["w_iq"]).reshape(DEPTH, 256, 256))
    wuk = np.asarray(inputs["w_uk"], dtype=np.float32)
    wukT = wuk.reshape(DEPTH, 128, 512).transpose(0, 2, 1)
    eo = np.zeros((DEPTH, 2, 4, 128, 128), np.float32)
    w5 = wukT.reshape(DEPTH, 4, 2, 64, 128)
    eo[:, 0, :, 0:64, :] = w5[:, :, 0]
    eo[:, 1, :, 64:128, :] = w5[:, :, 1]
    sh["w_ukT"] = f(eo.reshape(DEPTH, 1024, 128))
    wuv = np.asarray(inputs["w_uv"], dtype=np.float32)
    pad = np.zeros((DEPTH, 128, 8, 128), np.float32)
    for h in range(8):
        pad[:, :, h, (h % 2) * 64:(h % 2) * 64 + 64] = wuv[:, :, h, :]
    sh["w_uvp"] = f(pad.reshape(DEPTH, 128, 1024))

    def colT(a, nch):
        a = np.asarray(a, dtype=np.float32).reshape(DEPTH, nch, 128)
        return f(a.transpose(2, 0, 1).reshape(128, DEPTH * nch))
    sh["norm1T"] = colT(inputs["norm1"], 8)
    sh["norm2T"] = colT(inputs["norm2"], 8)
    sh["mem_normT"] = colT(inputs["mem_norm"], 8)
    sh["q_normT"] = colT(inputs["q_norm"], 2)
    sh["final_norm"] = f(np.asarray(inputs["final_norm"]).reshape(1, D))
    c = make_consts(S, NIT)
    sh["ident"] = c["ident"]
    sh["antiI"] = c["antiI"]
    sh["cos2"] = c["cos2"]
    sh["sin2"] = c["sin2"]
    sh["dmaskT"] = f(c["dmaskT"].reshape(128, 1024))
    sh["tabA"] = f(c["tabA"].reshape(128, 512))
    sh["tabB"] = f(c["tabB"].reshape(128, 512))
    sh["g64"] = f(c["g64"].reshape(128, 256))
    sh["kdec"] = c["kdec"]
    sh["oh"] = c["oh"]
    sh["pw"] = c["pw"]
    return sh


_CACHE = {}


def run(inputs, ncores, NSEQ, S, DEPTH, NIT=16, dbg=False, stop=None):
    key = (S, NSEQ, DEPTH, NIT, dbg, stop)
    if key not in _CACHE:
        _CACHE[key] = build_program(S, NSEQ, DEPTH, NIT, dbg, stop)
    nc, es, stats = _CACHE[key]
    sh = prep_shared(inputs, S, DEPTH, NIT)
    x = np.asarray(inputs["x"], dtype=np.float32)
    mem = np.asarray(inputs["mem"], dtype=np.float32)
    in_maps = []
    for c in range(ncores):
        m = dict(sh)
        m["x"] = np.ascontiguousarray(x[c * NSEQ:(c + 1) * NSEQ].reshape(NSEQ * S, D))
        m["mem"] = np.ascontiguousarray(mem[c * NSEQ:(c + 1) * NSEQ].reshape(NSEQ * 256, D))
        in_maps.append(m)
    res = run_bass_kernel_spmd(nc, in_maps, core_ids=list(range(ncores)))
    return res


def kernel(**inputs):
    B, S, _ = inputs["x"].shape
    NSEQ = B // NCORES
    res = run(inputs, NCORES, NSEQ, S, 2)
    outs = [r["out"].reshape(NSEQ, S, D) for r in res.results]
    return np.concatenate(outs, axis=0).astype(np.float32)
```
